# Optimizing a Trainium2 kernel written in Bass

```python
import math
import jax
import jax.numpy as jnp
from jax import lax
import numpy as np

D_MODEL = 1024
BATCH = 32
SEQ = 2048
DEPTH = 1

F32 = jnp.float32
MEM_LEN = 256
DIFF_HEADS = 8
DIFF_HEAD_DIM = 64
DIFF_V_DIM = 2 * DIFF_HEAD_DIM
ROPE_THETA = 500000.0
ROT_DIM = DIFF_HEAD_DIM // 4
Q_BLOCK = 128
GDN_HEADS = 8
GDN_HEAD_DIM = 128
CONV_WIDTH = 4
CHUNK = 64
MEM_HEADS = 4
MEM_HEAD_DIM = D_MODEL // MEM_HEADS
N_EXPERTS = 32
TOP_K = 4
EXPERT_FF = D_MODEL
SWIGLU_LIMIT = 7.0
SWIGLU_ALPHA = 1.702
EXPERT_BLOCK = 128
DEEPNORM_ALPHA = (2 * DEPTH) ** 0.25
DEEPNORM_BETA = (8 * DEPTH) ** -0.25
DIFF_QK_W = DIFF_HEADS * 2 * DIFF_HEAD_DIM
DIFF_V_W = DIFF_HEADS * DIFF_V_DIM
GDN_W = GDN_HEADS * GDN_HEAD_DIM
IN_SPLITS = (DIFF_QK_W, DIFF_QK_W, DIFF_V_W, GDN_W, GDN_W, GDN_W, GDN_W,
             GDN_HEADS, GDN_HEADS, D_MODEL, D_MODEL)
IN_PROJ_W = 3 * DIFF_QK_W + 4 * GDN_W + 2 * GDN_HEADS + 2 * D_MODEL

kernel_name = "hybrid_diffattn_gdn_moe_deepnorm"


def split_cols(t, sizes):
    outs, start = [], 0
    for s in sizes:
        outs.append(t[..., start:start + s])
        start += s
    return outs


def layer_norm(x, g, b, eps=1e-5):
    xf = x.astype(F32)
    mu = jnp.mean(xf, -1, keepdims=True)
    var = jnp.mean(jnp.square(xf - mu), -1, keepdims=True)
    return ((xf - mu) * lax.rsqrt(var + eps) * g.astype(F32) + b.astype(F32)).astype(x.dtype)


def rms_norm(x, g, eps=1e-6):
    xf = x.astype(F32)
    return (xf * lax.rsqrt(jnp.mean(xf * xf, -1, keepdims=True) + eps) * g.astype(F32)).astype(x.dtype)


def l2_normalize(t, eps=1e-6):
    return t * lax.rsqrt(jnp.sum(t * t, -1, keepdims=True) + eps)


def rope_tables(positions):
    inv_freq = jnp.power(ROPE_THETA, -jnp.arange(0, ROT_DIM, 2, dtype=F32) / ROT_DIM)
    ang = positions.astype(F32)[..., None] * inv_freq
    return jnp.cos(ang), jnp.sin(ang)


def apply_partial_rope(t, cos, sin):
    half = ROT_DIM // 2
    c = cos[:, :, None, None, :]
    s = sin[:, :, None, None, :]
    t1 = t[..., :half].astype(F32)
    t2 = t[..., half:ROT_DIM].astype(F32)
    rot = jnp.concatenate([t1 * c - t2 * s, t2 * c + t1 * s], -1).astype(t.dtype)
    return jnp.concatenate([rot, t[..., ROT_DIM:]], -1)


def differential_attention(q, k, v, cos, sin, lam_q1, lam_k1, lam_q2, lam_k2, subln_g, lambda_init):
    B, S, _ = q.shape
    q = apply_partial_rope(q.reshape(B, S, DIFF_HEADS, 2, DIFF_HEAD_DIM), cos, sin)
    k = apply_partial_rope(k.reshape(B, S, DIFF_HEADS, 2, DIFF_HEAD_DIM), cos, sin)
    q1, q2 = q[..., 0, :].transpose(0, 2, 1, 3), q[..., 1, :].transpose(0, 2, 1, 3)
    k1, k2 = k[..., 0, :].transpose(0, 2, 1, 3), k[..., 1, :].transpose(0, 2, 1, 3)
    v = v.reshape(B, S, DIFF_HEADS, DIFF_V_DIM).transpose(0, 2, 1, 3)
    lam = (jnp.exp(jnp.sum(lam_q1.astype(F32) * lam_k1.astype(F32)))
           - jnp.exp(jnp.sum(lam_q2.astype(F32) * lam_k2.astype(F32))) + lambda_init)
    scale = DIFF_HEAD_DIM ** -0.5
    nb = S // Q_BLOCK

    def to_blocks(t):
        return t.reshape(B, DIFF_HEADS, nb, Q_BLOCK, t.shape[-1]).transpose(2, 0, 1, 3, 4)

    k_pos = jnp.arange(S)

    def one_block(args):
        i, qb1, qb2 = args
        q_pos = i * Q_BLOCK + jnp.arange(Q_BLOCK)
        causal = k_pos[None, :] <= q_pos[:, None]

        def probs(qb, kk):
            s = jnp.einsum('bhqd,bhkd->bhqk', qb, kk).astype(F32) * scale
            return jax.nn.softmax(jnp.where(causal, s, -jnp.inf), axis=-1)

        p = probs(qb1, k1) - lam * probs(qb2, k2)
        return jnp.einsum('bhqk,bhkd->bhqd', p.astype(v.dtype), v)

    o = lax.map(one_block, (jnp.arange(nb), to_blocks(q1), to_blocks(q2)))
    o = o.transpose(1, 2, 0, 3, 4).reshape(B, DIFF_HEADS, S, DIFF_V_DIM)
    o = rms_norm(o, subln_g) * (1.0 - lambda_init)
    return o.transpose(0, 2, 1, 3).reshape(B, S, DIFF_V_W)


def chunked_gated_delta_rule(q, k, v, g, beta):
    B, S, H, dk = q.shape
    dv = v.shape[-1]
    N = S // CHUNK

    def to_chunks(t):
        t = t.reshape((B, N, CHUNK, H) + t.shape[3:])
        return jnp.moveaxis(jnp.moveaxis(t, 1, 0), 3, 2)

    q, k, v, g, beta = (to_chunks(t) for t in (q, k, v, g, beta))
    G = jnp.cumsum(g, axis=-1)
    idx = jnp.arange(CHUNK)
    incl = idx[:, None] >= idx[None, :]
    strict = idx[:, None] > idx[None, :]
    gap = G[..., :, None] - G[..., None, :]
    decay = jnp.where(incl, jnp.exp(jnp.where(incl, gap, 0.0)), 0.0)
    A = jnp.where(strict, jnp.einsum('nbhid,nbhjd->nbhij', k, k) * decay, 0.0) * beta[..., :, None]
    eG = jnp.exp(G)
    rhs = jnp.concatenate([v * beta[..., None], k * (beta * eG)[..., None]], -1)
    sol = lax.linalg.triangular_solve(A + jnp.eye(CHUNK, dtype=A.dtype), rhs,
                                      left_side=True, lower=True, unit_diagonal=True)
    u, w = sol[..., :dv], sol[..., dv:]
    qk = jnp.einsum('nbhid,nbhjd->nbhij', q, k) * decay
    q_dec = q * eG[..., None]
    k_dec = k * jnp.exp(G[..., -1:] - G)[..., None]
    g_last = eG[..., -1]

    def step(state, inp):
        u_n, w_n, qd_n, kd_n, qk_n, gl_n = inp
        v_new = u_n - jnp.einsum('bhck,bhkv->bhcv', w_n, state)
        o_n = jnp.einsum('bhck,bhkv->bhcv', qd_n, state) + jnp.einsum('bhij,bhjv->bhiv', qk_n, v_new)
        state = state * gl_n[..., None, None] + jnp.einsum('bhck,bhcv->bhkv', kd_n, v_new)
        return state, o_n

    state0 = jnp.zeros((B, H, dk, dv), F32)
    _, o = lax.scan(step, state0, (u, w, q_dec, k_dec, qk, g_last))
    return jnp.moveaxis(jnp.moveaxis(o, 2, 3), 0, 1).reshape(B, S, H, dv)


def gated_deltanet(q, k, v, z, b, a, conv_w, A_log, dt_bias, norm_g):
    B, S, _ = q.shape
    qkv = jnp.concatenate([q, k, v], -1)
    C = qkv.shape[-1]
    qkv = lax.conv_general_dilated(qkv, conv_w.astype(qkv.dtype)[:, None, :], (1,),
                                   [(CONV_WIDTH - 1, 0)],
                                   dimension_numbers=('NWC', 'WIO', 'NWC'),
                                   feature_group_count=C)
    qkv = jax.nn.silu(qkv)
    q, k, v = split_cols(qkv, (GDN_W, GDN_W, GDN_W))
    hd = (B, S, GDN_HEADS, GDN_HEAD_DIM)
    q = l2_normalize(q.reshape(hd).astype(F32)) * (GDN_HEAD_DIM ** -0.5)
    k = l2_normalize(k.reshape(hd).astype(F32))
    v = v.reshape(hd).astype(F32)
    beta = jax.nn.sigmoid(b.astype(F32))
    g = -jnp.exp(A_log.astype(F32)) * jax.nn.softplus(a.astype(F32) + dt_bias.astype(F32))
    o = chunked_gated_delta_rule(q, k, v, g, beta)
    o = rms_norm(o, norm_g) * jax.nn.silu(z.reshape(hd).astype(F32))
    return o.reshape(B, S, GDN_W).astype(z.dtype)


def memory_cross_attention(x, mem, w_q, w_k, w_v, w_o):
    B, S, _ = x.shape
    M = mem.shape[1]
    q = (x @ w_q).reshape(B, S, MEM_HEADS, MEM_HEAD_DIM)
    k = (mem @ w_k).reshape(B, M, MEM_HEADS, MEM_HEAD_DIM)
    v = (mem @ w_v).reshape(B, M, MEM_HEADS, MEM_HEAD_DIM)
    s = jnp.einsum('bshd,bmhd->bhsm', q, k).astype(F32) * (MEM_HEAD_DIM ** -0.5)
    p = jax.nn.softmax(s, axis=-1)
    o = jnp.einsum('bhsm,bmhd->bshd', p.astype(v.dtype), v).reshape(B, S, D_MODEL)
    return o @ w_o


def moe_ffn(x, w_router, b_router, w1, b1, w2, b2):
    B, S, D = x.shape
    T = B * S
    TK = T * TOP_K
    xf = x.reshape(T, D)
    logits = (xf @ w_router).astype(F32) + b_router.astype(F32)
    top_val, top_idx = lax.top_k(logits, TOP_K)
    gates = jax.nn.softmax(top_val, axis=-1)
    flat_e = top_idx.reshape(TK)
    order = jnp.argsort(flat_e)
    sorted_e = flat_e[order]
    counts = jnp.bincount(flat_e, length=N_EXPERTS)
    starts = jnp.cumsum(counts) - counts
    padded = (counts + EXPERT_BLOCK - 1) // EXPERT_BLOCK * EXPERT_BLOCK
    pad_ends = jnp.cumsum(padded)
    pad_starts = pad_ends - padded
    dest = pad_starts[sorted_e] + jnp.arange(TK) - starts[sorted_e]
    n_blocks = -(-TK // EXPERT_BLOCK) + N_EXPERTS
    R = n_blocks * EXPERT_BLOCK
    row_tok = jnp.full((R,), T, jnp.int32).at[dest].set((order // TOP_K).astype(jnp.int32))
    row_gate = jnp.zeros((R,), F32).at[dest].set(gates.reshape(TK)[order])
    block_e = jnp.minimum(jnp.searchsorted(pad_ends, jnp.arange(n_blocks) * EXPERT_BLOCK, side='right'),
                          N_EXPERTS - 1)
    x_pad = jnp.concatenate([xf, jnp.zeros((1, D), xf.dtype)], 0)

    def one_block(acc, inp):
        tok, gate, e = inp
        h = x_pad[tok] @ w1[e] + b1[e]
        h_gate, h_up = h[:, :EXPERT_FF], h[:, EXPERT_FF:]
        h_gate = jnp.minimum(h_gate, SWIGLU_LIMIT)
        h_up = jnp.clip(h_up, -SWIGLU_LIMIT, SWIGLU_LIMIT)
        act = h_gate * jax.nn.sigmoid(SWIGLU_ALPHA * h_gate) * (h_up + 1.0)
        y = (act @ w2[e] + b2[e]).astype(F32)
        return acc.at[tok].add(y * gate[:, None]), None

    acc0 = jnp.zeros((T + 1, D), F32)
    acc, _ = lax.scan(one_block, acc0, (row_tok.reshape(n_blocks, EXPERT_BLOCK),
                                         row_gate.reshape(n_blocks, EXPERT_BLOCK), block_e))
    return acc[:T].reshape(B, S, D).astype(x.dtype)


def setup_inputs(seed: int = 0) -> dict:
    key = jax.random.key(seed)
    ks = jax.random.split(key, 40)
    L, D = DEPTH, D_MODEL

    def nrm(k, shape, scale):
        return jax.random.normal(k, shape, F32) * scale

    dt = jnp.exp(jax.random.uniform(ks[11], (L, GDN_HEADS), F32, math.log(1e-3), math.log(1e-1)))
    return {
        'x': nrm(ks[0], (BATCH, SEQ, D), 1.0),
        'mem': nrm(ks[1], (BATCH, MEM_LEN, D), 1.0),
        'positions': jnp.broadcast_to(jnp.arange(SEQ, dtype=jnp.int32)[None, :], (BATCH, SEQ)),
        'w_in': nrm(ks[2], (L, D, IN_PROJ_W), D ** -0.5),
        'diff_lambda_q1': nrm(ks[3], (L, DIFF_HEAD_DIM), 0.1),
        'diff_lambda_k1': nrm(ks[4], (L, DIFF_HEAD_DIM), 0.1),
        'diff_lambda_q2': nrm(ks[5], (L, DIFF_HEAD_DIM), 0.1),
        'diff_lambda_k2': nrm(ks[6], (L, DIFF_HEAD_DIM), 0.1),
        'diff_subln_g': 1.0 + nrm(ks[7], (L, DIFF_V_DIM), 0.02),
        'w_diff_o': nrm(ks[8], (L, DIFF_V_W, D), DIFF_V_W ** -0.5),
        'gdn_conv_w': nrm(ks[9], (L, CONV_WIDTH, 3 * GDN_W), CONV_WIDTH ** -0.5),
        'gdn_A_log': jnp.log(jax.random.uniform(ks[10], (L, GDN_HEADS), F32, 1.0, 16.0)),
        'gdn_dt_bias': dt + jnp.log(-jnp.expm1(-dt)),
        'gdn_norm_g': 1.0 + nrm(ks[12], (L, GDN_HEAD_DIM), 0.02),
        'w_gdn_o': nrm(ks[13], (L, GDN_W, D), GDN_W ** -0.5),
        'w_mix_o': nrm(ks[14], (L, D, D), D ** -0.5 * DEEPNORM_BETA),
        'ln1_g': 1.0 + nrm(ks[15], (L, D), 0.02),
        'ln1_b': nrm(ks[16], (L, D), 0.01),
        'w_cq': nrm(ks[17], (L, D, D), D ** -0.5),
        'w_ck': nrm(ks[18], (L, D, D), D ** -0.5),
        'w_cv': nrm(ks[19], (L, D, D), D ** -0.5),
        'w_co': nrm(ks[20], (L, D, D), D ** -0.5 * DEEPNORM_BETA),
        'ln2_g': 1.0 + nrm(ks[21], (L, D), 0.02),
        'ln2_b': nrm(ks[22], (L, D), 0.01),
        'w_router': nrm(ks[23], (L, D, N_EXPERTS), D ** -0.5),
        'b_router': nrm(ks[24], (L, N_EXPERTS), 0.01),
        'w_exp_in': nrm(ks[25], (L, N_EXPERTS, D, 2 * EXPERT_FF), D ** -0.5),
        'b_exp_in': nrm(ks[26], (L, N_EXPERTS, 2 * EXPERT_FF), 0.01),
        'w_exp_out': nrm(ks[27], (L, N_EXPERTS, EXPERT_FF, D), EXPERT_FF ** -0.5 * DEEPNORM_BETA),
        'b_exp_out': nrm(ks[28], (L, N_EXPERTS, D), 0.01),
        'ln3_g': 1.0 + nrm(ks[29], (L, D), 0.02),
        'ln3_b': nrm(ks[30], (L, D), 0.01),
    }


def reference(x, mem, positions, w_in, diff_lambda_q1, diff_lambda_k1, diff_lambda_q2,
              diff_lambda_k2, diff_subln_g, w_diff_o, gdn_conv_w, gdn_A_log, gdn_dt_bias,
              gdn_norm_g, w_gdn_o, w_mix_o, ln1_g, ln1_b, w_cq, w_ck, w_cv, w_co, ln2_g, ln2_b,
              w_router, b_router, w_exp_in, b_exp_in, w_exp_out, b_exp_out, ln3_g, ln3_b):
    cos, sin = rope_tables(positions)
    for l in range(DEPTH):
        lambda_init = 0.8 - 0.6 * math.exp(-0.3 * l)
        proj = x @ w_in[l]
        (dq, dk, dv, gq, gk, gv, gz, gb, ga, gate_a, gate_b) = split_cols(proj, IN_SPLITS)
        y_diff = differential_attention(dq, dk, dv, cos, sin, diff_lambda_q1[l], diff_lambda_k1[l],
                                        diff_lambda_q2[l], diff_lambda_k2[l], diff_subln_g[l],
                                        lambda_init) @ w_diff_o[l]
        y_gdn = gated_deltanet(gq, gk, gv, gz, gb, ga, gdn_conv_w[l], gdn_A_log[l], gdn_dt_bias[l],
                               gdn_norm_g[l]) @ w_gdn_o[l]
        mixed = (jax.nn.sigmoid(gate_a) * y_diff + jax.nn.sigmoid(gate_b) * y_gdn) @ w_mix_o[l]
        x = layer_norm(DEEPNORM_ALPHA * x + mixed, ln1_g[l], ln1_b[l])
        y_mem = memory_cross_attention(x, mem, w_cq[l], w_ck[l], w_cv[l], w_co[l])
        x = layer_norm(DEEPNORM_ALPHA * x + y_mem, ln2_g[l], ln2_b[l])
        y_moe = moe_ffn(x, w_router[l], b_router[l], w_exp_in[l], b_exp_in[l], w_exp_out[l], b_exp_out[l])
        x = layer_norm(DEEPNORM_ALPHA * x + y_moe, ln3_g[l], ln3_b[l])
    return x
```

```python
import math
from contextlib import ExitStack
import numpy as np
import concourse.bass as bass
import concourse.mybir as mybir
from concourse.bass_utils import run_bass_kernel_spmd

F32 = mybir.dt.float32
BF16 = mybir.dt.bfloat16
I32 = mybir.dt.int32
ALU = mybir.AluOpType
AF = mybir.ActivationFunctionType
AX = mybir.AxisListType

ENGS = ['pe', 'act', 'dve', 'pool', 'sp']
D = 1024
S_LEN = 2048
NT = 16
MEM = 256
NCORES = 8
SEQ_PER_CORE = 4
INW = 9232
O_DQ, O_DK, O_DV, O_GQ, O_GK, O_GV, O_GZ, O_GB, O_GA, O_GTA, O_GTB = 0, 1024, 2048, 3072, 4096, 5120, 6144, 7168, 7176, 7184, 8208
LAMBDA_INIT = 0.2
ALPHA = 2.0 ** 0.25
PI = math.pi


class Res:
    __slots__ = ('name', 't', 'w', 'r', 'subs')

    def __init__(self, name, t=None):
        self.name = name
        self.t = t
        self.w = None
        self.r = {}
        self.subs = {}

    def sub(self, k):
        s = self.subs.get(k)
        if s is None:
            s = Res(f"{self.name}.{k}", self.t)
            self.subs[k] = s
        return s


class Sched:
    def __init__(self, nc):
        self.nc = nc
        self.stack = ExitStack()
        self.ops = {e: [] for e in ENGS}
        self.seen = {e: {} for e in ENGS}
        self.dma_cnt = {}
        self.halt = False

    def sb(self, name, shape, dtype):
        t = self.stack.enter_context(self.nc.sbuf_tensor(name, list(shape), dtype))
        return Res(name, t)

    def ps(self, name, shape, dtype):
        t = self.stack.enter_context(self.nc.psum_tensor(name, list(shape), dtype))
        return Res(name, t)

    def _deps(self, eng, reads, writes):
        deps = {}

        def add(tok):
            if tok is None:
                return
            k = (tok[0], tok[1])
            if deps.get(k, -1) < tok[2]:
                deps[k] = tok[2]
        for r in reads:
            add(r.w)
        for w in writes:
            add(w.w)
            for k, v in w.r.items():
                add((k[0], k[1], v))
        out = []
        seen = self.seen[eng]
        for k, v in deps.items():
            if k[0] == 'e' and k[1] == 'pe' and eng == 'pe':
                continue
            if seen.get(k, -1) >= v:
                continue
            seen[k] = v
            out.append((k[0], k[1], v))
        return out

    def _mark(self, tok, reads, writes):
        k = (tok[0], tok[1])
        for w in writes:
            w.w = tok
            w.r = {}
        for r in reads:
            if r.r.get(k, -1) < tok[2]:
                r.r[k] = tok[2]

    def op(self, eng, fn, reads=(), writes=()):
        if self.halt:
            return
        waits = self._deps(eng, reads, writes)
        idx = len(self.ops[eng])
        self.ops[eng].append([fn, waits, 'c', None, False])
        self._mark(('e', eng, idx), reads, writes)

    def dma(self, q, out, in_, key, reads=(), writes=(), **kw):
        self.dma_fn(q, (lambda e: e.dma_start(out=out, in_=in_, **kw)), key, reads, writes)

    def dma_fn(self, q, fn, key, reads=(), writes=()):
        if self.halt:
            return
        waits = self._deps(q, reads, writes)
        v = self.dma_cnt.get(key, 0) + 16
        self.dma_cnt[key] = v
        self.ops[q].append([fn, waits, 'd', key, False])
        self._mark(('d', key, v), reads, writes)

    def finish(self):
        nc = self.nc
        fin = [('d', key, v) for key, v in self.dma_cnt.items()]
        for e in ENGS:
            for o in self.ops[e]:
                for tok in o[1]:
                    if tok[0] == 'e':
                        self.ops[tok[1]][tok[2]][4] = True
        cum = {}
        for e in ENGS:
            c = 0
            arr = []
            for o in self.ops[e]:
                if o[2] == 'c' and o[4]:
                    c += 1
                arr.append(c)
            cum[e] = arr
        esem = {e: self.stack.enter_context(nc.semaphore(f"s_{e}")) for e in ENGS}
        dsem = {k: self.stack.enter_context(nc.semaphore(f"d_{i}")) for i, k in enumerate(self.dma_cnt)}
        self.n_sems = len(esem) + len(dsem)

        def emit(e, eng):
            for o in self.ops[e]:
                fn, waits, kind, key, marked = o
                for tok in waits:
                    if tok[0] == 'e':
                        eng.wait_ge(esem[tok[1]], cum[tok[1]][tok[2]])
                    else:
                        eng.wait_ge(dsem[tok[1]], tok[2])
                ins = fn(eng)
                if kind == 'd':
                    ins.then_inc(dsem[key], 16)
                elif marked:
                    ins.then_inc(esem[e], 1)
            if e == 'sp':
                for tok in fin:
                    eng.wait_ge(dsem[tok[1]], tok[2])

        with nc.Block() as block:
            @block.tensor
            def _(eng):
                emit('pe', eng)

            @block.scalar
            def _(eng):
                emit('act', eng)

            @block.vector
            def _(eng):
                emit('dve', eng)

            @block.gpsimd
            def _(eng):
                emit('pool', eng)

            @block.sync
            def _(eng):
                emit('sp', eng)
        self.stack.close()


def make_consts():
    c = {}
    idx = np.arange(128)
    c['ident'] = np.eye(128, dtype=np.float32)
    c['u_incl'] = (idx[:, None] <= idx[None, :]).astype(np.float32)
    c['u_strict'] = (idx[:, None] < idx[None, :]).astype(np.float32)
    c['l_gt'] = (idx[:, None] > idx[None, :]).astype(np.float32)
    c['ones'] = np.ones((128, 128), np.float32)
    inv_freq = np.power(500000.0, -np.arange(0, 16, 2, dtype=np.float32) / 16).astype(np.float32)
    cols = np.zeros((128, 4), np.float32)
    pm = np.zeros((128, 128), np.float32)
    for p in range(128):
        d = p % 64
        base = p - d
        if d < 8:
            cols[p, 0] = inv_freq[d]
            cols[p, 1] = -1.0
            cols[p, 2] = PI
            pm[base + d + 8, p] = 1.0
        elif d < 16:
            cols[p, 0] = inv_freq[d - 8]
            cols[p, 1] = 1.0
            cols[p, 2] = -PI
            pm[base + d - 8, p] = 1.0
        else:
            cols[p, 1] = 1.0
            cols[p, 2] = -PI
    c['rope_cols'] = cols
    c['rope_pm'] = pm
    return c


CONST_NAMES = ['ident', 'u_incl', 'u_strict', 'l_gt', 'ones', 'rope_pm']


class StopBuild(Exception):
    pass


def build(n_seq=SEQ_PER_CORE, stages=('diff',), dbg=(), stop_at=None):
    nc = bass.Bass("TRN2", target_bir_lowering=False)
    S = Sched(nc)

    def dram_in(name, shape, dt=F32):
        return nc.dram_tensor(name, list(shape), dt, kind="ExternalInput").ap()

    x_d = dram_in("x", [n_seq, S_LEN, D])
    mem_d = dram_in("mem", [n_seq, MEM, D])
    pos_d = dram_in("pos", [n_seq, S_LEN], I32)
    w_in = dram_in("w_in", [D, INW])
    lamv = dram_in("lamv", [4, 64])
    subln_g = dram_in("subln_g", [1, 128])
    w_diff_o = dram_in("w_diff_o", [D, D])
    cdr = {n: dram_in("c_" + n, [128, 128]) for n in CONST_NAMES}
    rope_cols_d = dram_in("c_rope_cols", [128, 4])
    out_d = nc.dram_tensor("out", [n_seq, S_LEN, D], F32, kind="ExternalOutput").ap()
    dbg_d = {}
    if stop_at is not None:
        dbg_d['stop'] = nc.dram_tensor("dbg_stop", [128, 512], F32, kind="ExternalOutput").ap()

    def stop(name, ap_fn, res):
        if stop_at == name:
            S.op('dve', lambda e: e.tensor_copy(dbgt.t[:, 0:ap_fn().shape[-1]], ap_fn()), reads=res, writes=[dbgt])
            S.dma('sp', dbg_d['stop'], dbgt.t[:], key='dbgt', reads=[dbgt])
            S.halt = True
    if 'ogdn' in dbg:
        dbg_d['ogdn'] = nc.dram_tensor("dbg_ogdn", [D, S_LEN], F32, kind="ExternalOutput").ap()
    if 'ondiff' in dbg:
        dbg_d['ondiff'] = nc.dram_tensor("dbg_ondiff", [D, S_LEN], F32, kind="ExternalOutput").ap()

    dbgt = S.sb("dbgt", [128, 512], F32)
    S.op('dve', lambda e: e.memset(dbgt.t[:], 0.0), writes=[dbgt])
    cf = {n: S.sb("cf_" + n, [128, 128], F32) for n in CONST_NAMES}
    cb = {n: S.sb("cb_" + n, [128, 128], BF16) for n in ['ident', 'ones', 'rope_pm', 'u_incl', 'u_strict']}
    for n in CONST_NAMES:
        S.dma('sp', cf[n].t[:], cdr[n], key='c_' + n, writes=[cf[n]])
    for n in cb:
        S.op('dve', lambda e, n=n: e.tensor_copy(cb[n].t[:], cf[n].t[:]), reads=[cf[n]], writes=[cb[n]])
    rcols = S.sb("rcols", [128, 4], F32)
    S.dma('sp', rcols.t[:], rope_cols_d, key='c_rcols', writes=[rcols])

    stop('c0', lambda: cb['rope_pm'].t[:], [cb['rope_pm']])
    lamt = S.sb("lamt", [128, 4, 64], F32)
    S.dma('sp', lamt.t[:], lamv.rearrange("(o a) b -> o a b", o=1).broadcast_to([128, 4, 64]), key='c_lam', writes=[lamt])
    lam_s = S.sb("lam_s", [128, 8], F32)
    lam_j = S.sb("lam_j", [128, 64], F32)
    for i in range(2):
        S.op('dve', lambda e, i=i: e.tensor_tensor(lam_j.t[:], lamt.t[:, 2 * i, :], lamt.t[:, 2 * i + 1, :], ALU.mult),
             reads=[lamt], writes=[lam_j])
        S.op('dve', lambda e, i=i: e.reduce_sum(lam_s.t[:, i:i + 1], lam_j.t[:], axis=AX.X), reads=[lam_j], writes=[lam_s])
    S.op('act', lambda e: e.activation(lam_s.t[:, 2:4], lam_s.t[:, 0:2], AF.Exp), reads=[lam_s], writes=[lam_s])
    S.op('dve', lambda e: e.tensor_tensor(lam_s.t[:, 4:5], lam_s.t[:, 3:4], lam_s.t[:, 2:3], ALU.subtract), reads=[lam_s], writes=[lam_s])
    S.op('dve', lambda e: e.tensor_scalar(lam_s.t[:, 5:6], lam_s.t[:, 4:5], -LAMBDA_INIT, None, ALU.add), reads=[lam_s], writes=[lam_s])
    NEG_LAM = lam_s.t[:, 5:6]
    stop('lam', lambda: lam_s.t[:, 0:8], [lam_s])
    gsub = S.sb("gsub", [128, 128], F32)
    S.dma('sp', gsub.t[:], subln_g.broadcast_to([128, 128]), key='c_gsub', writes=[gsub])
    S.op('dve', lambda e: e.tensor_scalar(gsub.t[:], gsub.t[:], 1.0 - LAMBDA_INIT, None, ALU.mult), reads=[gsub], writes=[gsub])

    stop('gsub', lambda: gsub.t[:], [gsub])
    xT = S.sb("xT", [128, 8, S_LEN], BF16)
    onT = S.sb("onT", [128, 8, S_LEN], BF16)
    xf = [S.sb(f"xf{i}", [128, D], F32) for i in range(1)]
    xb = [S.sb(f"xb{i}", [128, D], BF16) for i in range(1)]
    NWB = 2
    wbuf = [S.sb(f"wbuf{i}", [128, 8, 512], BF16) for i in range(NWB)]
    wctr = [0]
    PB = [S.ps(f"pb{i}", [128, 512], F32) for i in range(8)]

    def load_w(parts):
        wb = wbuf[wctr[0] % NWB]
        wctr[0] += 1
        for ap, off in parts:
            n = ap.shape[1]
            S.dma('pool', wb.t[:, :, off:off + n], ap.rearrange("(c p) n -> p c n", p=128),
                  key=wb.name, writes=[wb])
        return wb

    posi = S.sb("posi", [128, S_LEN + 8], I32)
    ang = S.sb("ang", [128, S_LEN + 8], F32)
    cosT = S.sb("cosT", [128, S_LEN], BF16)
    sinT = S.sb("sinT", [128, S_LEN], BF16)
    rr_a = S.sb("rr_a", [128, S_LEN + 8], F32)
    eps6 = S.sb("eps6", [128, 1], F32)
    S.op('dve', lambda e: e.memset(eps6.t[:], 1e-6), writes=[eps6])

    qraw = S.sb("qraw", [128, S_LEN], BF16)
    qT = S.sb("qT", [128, S_LEN], BF16)
    kT = S.sb("kT", [128, S_LEN], BF16)
    kTm = [S.sb(f"kTm{i}", [128, S_LEN], BF16) for i in range(2)]
    cmask = S.sb("cmask", [128, 2], F32)
    S.op('dve', lambda e: e.memset(cmask.t[:], 0.0), writes=[cmask])
    S.op('dve', lambda e: e.memset(cmask.t[0:64, 0:1], 1.0), writes=[cmask])
    S.op('dve', lambda e: e.memset(cmask.t[64:128, 1:2], 1.0), writes=[cmask])
    vaug = S.sb("vaug", [128, NT, 132], BF16)
    S.op('dve', lambda e: e.memset(vaug.t[:], 1.0), writes=[vaug])
    rt1 = S.sb("rt1", [128, 512], F32)
    rt2 = S.sb("rt2", [128, 512], F32)
    PT = [S.sb(f"PT{i}", [128, 512], BF16) for i in range(2)]
    otmp = S.sb("otmp", [128, 128], F32)
    ocmb = S.sb("ocmb", [128, 128], F32)
    ojunk = S.sb("ojunk", [128, 128], F32)
    ontok = S.sb("ontok", [128, 128], BF16)
    osm = S.sb("osm", [128, 8], F32)

    def bview(res, dt, shape_str=None, **kw):
        ap = res.t[:]
        if len(ap.shape) == 3:
            ap = ap.rearrange("p a b -> p (a b)")
        ap = ap.bitcast(dt)
        if shape_str:
            ap = ap.rearrange(shape_str, **kw)
        return ap

    if 'gdn' in stages:
        conv_wT = dram_in("conv_wT", [128, 96])
        gsc_d = dram_in("gdn_sc", [2, 8])
        gnorm_d = dram_in("gdn_norm_g", [1, 128])
        gdnT = S.sb("gdnT", [128, 8, S_LEN], BF16)
        cw = S.sb("cw", [128, 96], F32)
        S.dma('sp', cw.t[:], conv_wT, key='c_cw', writes=[cw])
        gsc = S.sb("gsc", [128, 2, 8], F32)
        S.dma('sp', gsc.t[:], gsc_d.rearrange("(o a) b -> o a b", o=1).broadcast_to([128, 2, 8]), key='c_gsc', writes=[gsc])
        negA = S.sb("negA", [128, 8], F32)
        S.op('act', lambda e: e.activation(negA.t[:], gsc.t[:, 0, :], AF.Exp), reads=[gsc], writes=[negA])
        S.op('dve', lambda e: e.tensor_scalar(negA.t[:], negA.t[:], -1.0, None, ALU.mult), reads=[negA], writes=[negA])
        gnorm = S.sb("gnorm", [128, 128], F32)
        S.dma('sp', gnorm.t[:], gnorm_d.broadcast_to([128, 128]), key='c_gnorm', writes=[gnorm])
        one1 = S.sb("one1", [128, 1], F32)
        S.op('dve', lambda e: e.memset(one1.t[:], 1.0), writes=[one1])
        sc_ba = S.sb("sc_ba", [128, NT, 16], F32)
        sc_beta = S.sb("sc_beta", [128, NT, 8], F32)
        sc_nbeta = S.sb("sc_nbeta", [128, NT, 8], F32)
        sc_g = S.sb("sc_g", [128, NT, 8], F32)
        sc_eG = S.sb("sc_eG", [128, NT, 8], F32)
        sc_neG = S.sb("sc_neG", [128, NT, 8], F32)
        sc_dk = S.sb("sc_dk", [128, NT, 8], F32)
        sc_gl = S.sb("sc_gl", [128, NT, 8], F32)
        sc_tmp = S.sb("sc_tmp", [128, NT, 8], F32)
        Gm = S.sb("Gm", [128, 128], F32)
        dec = S.sb("dec", [128, 128], F32)
        decI = S.sb("decI", [128, 128], BF16)
        decS = S.sb("decS", [128, 128], BF16)
        Yb = [S.sb(f"Yb{i}", [128, 128], BF16) for i in range(2)]
        Zb = [S.sb(f"Zb{i}", [128, 128], BF16) for i in range(2)]
        Tb = [S.sb(f"Tb{i}", [128, 128], BF16) for i in range(2)]
        Ttall = S.sb("Ttall", [128, 1, 128], BF16)
        QKDall = S.sb("QKDall", [128, 1, 128], BF16)
        zs = S.sb("zs", [128, 1, 128], BF16)
        st_f = S.sb("st_f", [128, 128], F32)
        st_b = S.sb("st_b", [128, 128], BF16)
        Rt = S.sb("Rt", [128, 128], BF16)
        vnew = S.sb("vnew", [128, 128], BF16)
        gtmp = S.sb("gtmp", [128, 128], F32)
        go = S.sb("go", [128, 128], F32)
        gog = S.sb("gog", [128, 128], F32)
        gob = S.sb("gob", [128, 128], BF16)
        gsm = S.sb("gsm", [128, 8], F32)
        RW = S_LEN + 8
        raws = [(ang, 0), (ang, 1), (posi, 0)]
        cacc = rr_a
        sil = rr_a
        qnT, knT, vsT, sqb = qT, kT, qraw, kTm[0]
        kdec = kTm[1]
        vtok = vaug

    def gdn_stage(s):
        pT = PB[6].t[:].bitcast(BF16)
        wba = load_w([(w_in[:, O_GB:O_GB + 16], 0)])
        for t in range(NT):
            for c in range(8):
                S.op('pe', lambda e, c=c, t=t: e.matmul(PB[7].t[:, t * 16:(t + 1) * 16], xT.t[:, c, t * 128:(t + 1) * 128], wba.t[:, c, 0:16],
                                                        start=(c == 0), stop=(c == 7)), reads=[wba, xT], writes=[PB[7]])
        S.op('act', lambda e: e.copy(sc_ba.t[:], PB[7].t[:, 0:256].rearrange("p (a b) -> p a b", a=NT)), reads=[PB[7]], writes=[sc_ba])
        S.op('act', lambda e: e.activation(sc_tmp.t[:], sc_ba.t[:, :, 0:8], AF.Exp, scale=-1.0), reads=[sc_ba], writes=[sc_tmp])
        S.op('dve', lambda e: e.tensor_scalar(sc_tmp.t[:], sc_tmp.t[:], 1.0, None, ALU.add), reads=[sc_tmp], writes=[sc_tmp])
        S.op('dve', lambda e: e.reciprocal(sc_beta.t[:], sc_tmp.t[:]), reads=[sc_tmp], writes=[sc_beta])
        S.op('dve', lambda e: e.tensor_scalar(sc_nbeta.t[:], sc_beta.t[:], -1.0, None, ALU.mult), reads=[sc_beta], writes=[sc_nbeta])
        S.op('dve', lambda e: e.tensor_tensor(sc_tmp.t[:], sc_ba.t[:, :, 8:16], gsc.t[:, 1:2, :].broadcast_to([128, NT, 8]), ALU.add),
             reads=[sc_ba, gsc], writes=[sc_tmp])
        S.op('act', lambda e: e.activation(sc_tmp.t[:], sc_tmp.t[:], AF.Exp), reads=[sc_tmp], writes=[sc_tmp])
        S.op('act', lambda e: e.activation(sc_tmp.t[:], sc_tmp.t[:], AF.Ln, bias=one1.t[:, 0:1], scale=1.0), reads=[sc_tmp, one1], writes=[sc_tmp])
        S.op('dve', lambda e: e.tensor_tensor(sc_g.t[:], sc_tmp.t[:], negA.t[:].unsqueeze(1).broadcast_to([128, NT, 8]), ALU.mult),
             reads=[sc_tmp, negA], writes=[sc_g])
        gflat = sc_g.t[:].rearrange("p a b -> p (a b)")
        S.op('pe', lambda e: e.matmul(PB[7].t[:, 0:128], cf['u_incl'].t[:], gflat, start=True, stop=True), reads=[cf['u_incl'], sc_g], writes=[PB[7]])
        S.op('pe', lambda e: e.matmul(PB[7].t[:, 128:256], cf['ones'].t[:], gflat, start=True, stop=True), reads=[cf['ones'], sc_g], writes=[PB[7]])
        Gps = PB[7].t[:, 0:128].rearrange("p (a b) -> p a b", a=NT)
        GLps = PB[7].t[:, 128:256].rearrange("p (a b) -> p a b", a=NT)
        S.op('act', lambda e: e.activation(sc_eG.t[:], Gps, AF.Exp), reads=[PB[7]], writes=[sc_eG])
        S.op('dve', lambda e: e.tensor_scalar(sc_neG.t[:], sc_eG.t[:], -1.0, None, ALU.mult), reads=[sc_eG], writes=[sc_neG])
        S.op('act', lambda e: e.activation(sc_gl.t[:], GLps, AF.Exp), reads=[PB[7]], writes=[sc_gl])
        S.op('act', lambda e: e.copy(sc_tmp.t[:], Gps), reads=[PB[7]], writes=[sc_tmp])
        S.op('dve', lambda e: e.tensor_tensor(sc_tmp.t[:], GLps, sc_tmp.t[:], ALU.subtract), reads=[PB[7], sc_tmp], writes=[sc_tmp])
        S.op('act', lambda e: e.activation(sc_dk.t[:], sc_tmp.t[:], AF.Exp), reads=[sc_tmp], writes=[sc_dk])
        for r, hf in raws:
            S.op('pool', lambda e, r=r, hf=hf: e.memset(r.t[:].bitcast(BF16)[:, hf * RW:hf * RW + 8], 0.0), writes=[r])

        for h in range(8):
            wb = load_w([(w_in[:, O_GQ + h * 128:O_GQ + (h + 1) * 128], 0), (w_in[:, O_GK + h * 128:O_GK + (h + 1) * 128], 128),
                         (w_in[:, O_GV + h * 128:O_GV + (h + 1) * 128], 256), (w_in[:, O_GZ + h * 128:O_GZ + (h + 1) * 128], 384)])
            for j in range(3):
                raw, hf = raws[j]
                rv = raw.t[:].bitcast(BF16)[:, hf * RW:(hf + 1) * RW]
                for tb in range(4):
                    pj = PB[tb % 2]
                    tsl = slice(tb * 512, (tb + 1) * 512)
                    for c in range(8):
                        S.op('pe', lambda e, c=c, pj=pj, tsl=tsl, j=j, wb=wb: e.matmul(
                            pj.t[:], wb.t[:, c, j * 128:(j + 1) * 128], xT.t[:, c, tsl], start=(c == 0), stop=(c == 7)),
                            reads=[wb, xT], writes=[pj])
                    S.op('act', lambda e, pj=pj, tb=tb, rv=rv: e.copy(rv[:, 3 + tb * 512:3 + (tb + 1) * 512], pj.t[:]), reads=[pj], writes=[raw])
                cwc = lambda w, j=j, h=h: cw.t[:, (j * 8 + h) * 4 + w:(j * 8 + h) * 4 + w + 1]
                S.op('dve', lambda e, rv=rv, cwc=cwc: e.tensor_scalar(cacc.t[:, 0:S_LEN], rv[:, 3:3 + S_LEN], cwc(3), None, ALU.mult), reads=[raw, cw], writes=[cacc])
                for w_, eng in ((2, 'dve'), (1, 'dve'), (0, 'dve')):
                    S.op(eng, lambda e, rv=rv, cwc=cwc, w_=w_: e.scalar_tensor_tensor(cacc.t[:, 0:S_LEN], rv[:, w_:w_ + S_LEN], cwc(w_), cacc.t[:, 0:S_LEN], ALU.mult, ALU.add),
                         reads=[raw, cw, cacc], writes=[cacc])
                if j == 2:
                    S.op('act', lambda e: e.activation(vsT.t[:], cacc.t[:, 0:S_LEN], AF.Silu), reads=[cacc], writes=[vsT.sub(0), vsT.sub(1), vsT.sub(2), vsT.sub(3)])
                else:
                    dstT = qnT if j == 0 else knT
                    qscale = (128.0 ** -0.5) if j == 0 else 1.0
                    S.op('act', lambda e: e.activation(sil.t[:, 0:S_LEN], cacc.t[:, 0:S_LEN], AF.Silu), reads=[cacc], writes=[sil])
                    S.op('act', lambda e: e.activation(sqb.t[:], sil.t[:, 0:S_LEN], AF.Square), reads=[sil], writes=[sqb])
                    for tb in range(4):
                        tsl = slice(tb * 512, (tb + 1) * 512)
                        S.op('pe', lambda e, tsl=tsl: e.matmul(PB[7].t[:], cb['ones'].t[:], sqb.t[:, tsl], start=True, stop=True),
                             reads=[cb['ones'], sqb], writes=[PB[7]])
                        S.op('act', lambda e: e.activation(rt1.t[:], PB[7].t[:], AF.Ln, bias=eps6.t[:, 0:1], scale=1.0), reads=[PB[7], eps6], writes=[rt1])
                        S.op('act', lambda e: e.activation(rt1.t[:], rt1.t[:], AF.Exp, scale=-0.5), reads=[rt1], writes=[rt1])
                        S.op('dve', lambda e, tsl=tsl, dstT=dstT, qscale=qscale: e.scalar_tensor_tensor(
                            dstT.t[:, tsl], sil.t[:, tsl], qscale, rt1.t[:], ALU.mult, ALU.mult), reads=[sil, rt1], writes=[dstT.sub(tb)])
            for t in range(NT):
                tsl = slice(t * 128, (t + 1) * 128)
                S.op('pe', lambda e, tsl=tsl: e.transpose(pT[:, 0:128], vsT.t[:, tsl], cb['ident'].t[:]), reads=[vsT.sub(t // 4), cb['ident']], writes=[PB[6]])
                S.op('act', lambda e, t=t: e.copy(vtok.t[:, t, 0:128], pT[:, 0:128]), reads=[PB[6]], writes=[vtok])
                S.op('pe', lambda e, tsl=tsl: e.transpose(pT[:, 128:256], knT.t[:, tsl], cb['ident'].t[:]), reads=[knT.sub(t // 4), cb['ident']], writes=[PB[6]])
                S.op('act', lambda e, t=t, h=h, tsl=tsl: e.activation(kdec.t[:, tsl], pT[:, 128:256], AF.Copy, scale=sc_dk.t[:, t, h:h + 1]),
                     reads=[PB[6], sc_dk], writes=[kdec])
            S.op('dve', lambda e: e.memset(st_f.t[:], 0.0), writes=[st_f])
            S.op('dve', lambda e: e.memset(st_b.t[:], 0.0), writes=[st_b])
            for c in range(NT):
                tsl = slice(c * 128, (c + 1) * 128)
                S.op('dve', lambda e, c=c, h=h: e.tensor_scalar(Gm.t[:], cf['u_incl'].t[:], sc_g.t[:, c, h:h + 1], None, ALU.mult),
                     reads=[cf['u_incl'], sc_g], writes=[Gm])
                S.op('pe', lambda e: e.matmul(PB[0].t[:, 0:128], cf['l_gt'].t[:], Gm.t[:], start=True, stop=True), reads=[cf['l_gt'], Gm], writes=[PB[0]])
                S.op('act', lambda e: e.activation(dec.t[:], PB[0].t[:, 0:128], AF.Exp), reads=[PB[0]], writes=[dec])
                S.op('pool', lambda e: e.tensor_tensor(decI.t[:], dec.t[:], cf['u_incl'].t[:], ALU.mult), reads=[dec, cf['u_incl']], writes=[decI])
                S.op('pool', lambda e: e.tensor_tensor(decS.t[:], dec.t[:], cf['u_strict'].t[:], ALU.mult), reads=[dec, cf['u_strict']], writes=[decS])
                S.op('pe', lambda e, tsl=tsl: e.matmul(PB[1].t[:, 0:128], knT.t[:, tsl], knT.t[:, tsl], start=True, stop=True), reads=[knT.sub(c // 4)], writes=[PB[1]])
                S.op('pe', lambda e, tsl=tsl: e.matmul(PB[1].t[:, 128:256], knT.t[:, tsl], qnT.t[:, tsl], start=True, stop=True),
                     reads=[knT.sub(c // 4), qnT.sub(c // 4)], writes=[PB[1]])
                Y, Z, T = Yb[0], Zb[0], Tb[0]
                S.op('dve', lambda e, c=c, h=h, Y=Y: e.scalar_tensor_tensor(Y.t[:], PB[1].t[:, 0:128], sc_nbeta.t[:, c, h:h + 1], decS.t[:], ALU.mult, ALU.mult),
                     reads=[PB[1], sc_nbeta, decS], writes=[Y])
                S.op('dve', lambda e, c=c: e.tensor_tensor(QKDall.t[:, 0, :], PB[1].t[:, 128:256], decI.t[:], ALU.mult), reads=[PB[1], decI], writes=[QKDall])
                S.op('pe', lambda e, Y=Y: e.transpose(pT[:, 0:128], Y.t[:], cb['ident'].t[:]), reads=[Y, cb['ident']], writes=[PB[6]])
                S.op('act', lambda e, Z=Z: e.copy(Z.t[:], pT[:, 0:128]), reads=[PB[6]], writes=[Z])
                S.op('pool', lambda e, Y=Y, T=T: e.tensor_tensor(T.t[:], Y.t[:], cb['ident'].t[:], ALU.add), reads=[Y, cb['ident']], writes=[T])
                cur = 0
                for n in range(1, 7):
                    Y, Z, T = Yb[cur], Zb[cur], Tb[cur]
                    Y2, Z2, T2 = Yb[1 - cur], Zb[1 - cur], Tb[1 - cur]
                    S.op('pe', lambda e, Y=Y, Z=Z: e.matmul(PB[2].t[:, 0:128], Y.t[:], Z.t[:], start=True, stop=True), reads=[Y, Z], writes=[PB[2]])
                    S.op('act', lambda e, Z2=Z2: e.copy(Z2.t[:], PB[2].t[:, 0:128]), reads=[PB[2]], writes=[Z2])
                    if n < 6:
                        S.op('pe', lambda e, Y=Y, Z=Z: e.matmul(PB[3].t[:, 0:128], Z.t[:], Y.t[:], start=True, stop=True), reads=[Y, Z], writes=[PB[3]])
                        S.op('dve', lambda e, Y2=Y2: e.tensor_copy(Y2.t[:], PB[3].t[:, 0:128]), reads=[PB[3]], writes=[Y2])
                    S.op('pe', lambda e, Z2=Z2, T=T: e.matmul(PB[0].t[:, 0:128], Z2.t[:], T.t[:], start=True, stop=True), reads=[Z2, T], writes=[PB[0]])
                    if n < 6:
                        S.op('dve', lambda e, T=T, T2=T2: e.tensor_tensor(T2.t[:], PB[0].t[:, 0:128], T.t[:], ALU.add), reads=[PB[0], T], writes=[T2])
                    else:
                        S.op('dve', lambda e, T=T, c=c: e.tensor_tensor(Ttall.t[:, 0, :], PB[0].t[:, 0:128], T.t[:], ALU.add), reads=[PB[0], T], writes=[Ttall])
                    cur = 1 - cur
                for kc in range(8):
                    S.op('pe', lambda e, kc=kc, tsl=tsl, wb=wb: e.matmul(PB[7].t[:, 0:128], xT.t[:, kc, tsl], wb.t[:, kc, 384:512], start=(kc == 0), stop=(kc == 7)),
                         reads=[wb, xT], writes=[PB[7]])
                S.op('act', lambda e: e.activation(zs.t[:, 0, :], PB[7].t[:, 0:128], AF.Silu), reads=[PB[7]], writes=[zs])
                S.op('pe', lambda e, tsl=tsl: e.matmul(PB[2].t[:, 0:128], knT.t[:, tsl], st_b.t[:], start=True, stop=True), reads=[knT.sub(c // 4), st_b], writes=[PB[2]])
                S.op('dve', lambda e, c=c, h=h: e.scalar_tensor_tensor(Rt.t[:], PB[2].t[:, 0:128], sc_neG.t[:, c, h:h + 1], vtok.t[:, c, 0:128], ALU.mult, ALU.add),
                     reads=[PB[2], sc_neG, vtok], writes=[Rt])
                S.op('pe', lambda e, c=c: e.matmul(PB[3].t[:, 0:128], Ttall.t[:, 0, :], Rt.t[:], start=True, stop=True), reads=[Ttall, Rt], writes=[PB[3]])
                S.op('act', lambda e, c=c, h=h: e.activation(vnew.t[:], PB[3].t[:, 0:128], AF.Copy, scale=sc_beta.t[:, c, h:h + 1]), reads=[PB[3], sc_beta], writes=[vnew])
                S.op('pe', lambda e, tsl=tsl: e.matmul(PB[0].t[:, 0:128], qnT.t[:, tsl], st_b.t[:], start=True, stop=True), reads=[qnT.sub(c // 4), st_b], writes=[PB[0]])
                S.op('pe', lambda e, c=c: e.matmul(PB[1].t[:, 0:128], QKDall.t[:, 0, :], vnew.t[:], start=True, stop=True), reads=[QKDall, vnew], writes=[PB[1]])
                S.op('act', lambda e, c=c, h=h: e.activation(gtmp.t[:], PB[0].t[:, 0:128], AF.Copy, scale=sc_eG.t[:, c, h:h + 1]), reads=[PB[0], sc_eG], writes=[gtmp])
                S.op('dve', lambda e: e.tensor_tensor(go.t[:], PB[1].t[:, 0:128], gtmp.t[:], ALU.add), reads=[PB[1], gtmp], writes=[go])
                S.op('pe', lambda e, tsl=tsl: e.matmul(PB[2].t[:, 128:256], kdec.t[:, tsl], vnew.t[:], start=True, stop=True), reads=[kdec, vnew], writes=[PB[2]])
                S.op('dve', lambda e, c=c, h=h: e.scalar_tensor_tensor(st_f.t[:], st_f.t[:], sc_gl.t[:, c, h:h + 1], PB[2].t[:, 128:256], ALU.mult, ALU.add),
                     reads=[st_f, sc_gl, PB[2]], writes=[st_f])
                S.op('pool', lambda e: e.tensor_copy(st_b.t[:], st_f.t[:]), reads=[st_f], writes=[st_b])
                S.op('act', lambda e: e.activation(gog.t[:], go.t[:], AF.Square, accum_out=gsm.t[:, 0:1]), reads=[go], writes=[gog, gsm])
                S.op('act', lambda e: e.activation(gsm.t[:, 1:2], gsm.t[:, 0:1], AF.Ln, bias=eps6.t[:, 0:1], scale=1.0 / 128), reads=[gsm, eps6], writes=[gsm])
                S.op('act', lambda e: e.activation(gsm.t[:, 2:3], gsm.t[:, 1:2], AF.Exp, scale=-0.5), reads=[gsm], writes=[gsm])
                S.op('dve', lambda e: e.scalar_tensor_tensor(gog.t[:], go.t[:], gsm.t[:, 2:3], gnorm.t[:], ALU.mult, ALU.mult), reads=[go, gsm, gnorm], writes=[gog])
                S.op('pool', lambda e, c=c: e.tensor_tensor(gob.t[:], gog.t[:], zs.t[:, 0, :], ALU.mult), reads=[gog, zs], writes=[gob])
                S.op('pe', lambda e: e.transpose(pT[:, 256:384], gob.t[:], cb['ident'].t[:]), reads=[gob, cb['ident']], writes=[PB[6]])
                S.op('act', lambda e, h=h, tsl=tsl: e.copy(gdnT.t[:, h, tsl], pT[:, 256:384]), reads=[PB[6]], writes=[gdnT])


    x1res = Res('x1res'); x2res = Res('x2res'); xT_act = Res('xT_act')
    if 'mix' in stages:
        w_gdn_o = dram_in("w_gdn_o", [D, D]); w_mix_o = dram_in("w_mix_o", [D, D])
        lnp_d = dram_in("lnp", [6, D])
        w_cq = dram_in("w_cq", [D, D]); w_ck = dram_in("w_ck", [D, D]); w_cv = dram_in("w_cv", [D, D]); w_co = dram_in("w_co", [D, D])
        w_router = dram_in("w_router", [D, 32]); b_router = dram_in("b_router", [1, 32])
        w_e1 = dram_in("w_e1", [32, D, 2048]); b1T_d = dram_in("b1T", [128, 512])
        w_e2 = dram_in("w_e2", [32, D, D]); b_e2 = dram_in("b_e2", [32, D])
        x1_d = nc.dram_tensor("x1_scr", [n_seq * S_LEN, D], F32, kind=("ExternalOutput" if "x12" in dbg else "Internal")).ap()
        x2_d = nc.dram_tensor("x2_scr", [n_seq * S_LEN, D], F32, kind=("ExternalOutput" if "x12" in dbg else "Internal")).ap()
        lsm = S.sb("lsm", [128, 8], F32)
        eps5 = S.sb("eps5", [128, 1], F32)
        S.op('dve', lambda e: e.memset(eps5.t[:], 1e-5), writes=[eps5])
        gated = rr_a.t[:].bitcast(BF16)[:, 0:4096].rearrange("p (a b) -> p a b", a=8)
        lny = ang.t[:, 0:1024]
        lno = ang.t[:, 1024:2048]
        pfv = posi.t[:].bitcast(F32)
        lng = pfv[:, 0:1024]
        lnb = pfv[:, 1024:2048]

    def load_ln(i):
        S.dma('sp', lng, lnp_d[2 * i:2 * i + 1, :].broadcast_to([128, D]), key='lnp', writes=[posi])
        S.dma('sp', lnb, lnp_d[2 * i + 1:2 * i + 2, :].broadcast_to([128, D]), key='lnp', writes=[posi])

    def layer_norm():
        S.op('act', lambda e: e.activation(lno, lny, AF.Copy, accum_out=lsm.t[:, 0:1]), reads=[ang], writes=[ang, lsm])
        S.op('dve', lambda e: e.tensor_scalar(lsm.t[:, 1:2], lsm.t[:, 0:1], -1.0 / D, None, ALU.mult), reads=[lsm], writes=[lsm])
        S.op('dve', lambda e: e.tensor_scalar(lny, lny, lsm.t[:, 1:2], None, ALU.add), reads=[ang, lsm], writes=[ang])
        S.op('act', lambda e: e.activation(lno, lny, AF.Square, accum_out=lsm.t[:, 2:3]), reads=[ang], writes=[ang, lsm])
        S.op('act', lambda e: e.activation(lsm.t[:, 3:4], lsm.t[:, 2:3], AF.Ln, bias=eps5.t[:, 0:1], scale=1.0 / D), reads=[lsm, eps5], writes=[lsm])
        S.op('act', lambda e: e.activation(lsm.t[:, 4:5], lsm.t[:, 3:4], AF.Exp, scale=-0.5), reads=[lsm], writes=[lsm])
        S.op('dve', lambda e: e.scalar_tensor_tensor(lno, lny, lsm.t[:, 4:5], lng, ALU.mult, ALU.mult), reads=[ang, lsm, posi], writes=[ang])
        S.op('pool', lambda e: e.tensor_tensor(lno, lno, lnb, ALU.add), reads=[ang, posi], writes=[ang])

    def to_xT(tile_idx):
        pT = PB[6].t[:].bitcast(BF16)
        b = xb[0]
        S.op('act', lambda e: e.copy(b.t[:], lno), reads=[ang], writes=[b])
        for c in range(8):
            S.op('pe', lambda e, c=c: e.transpose(pT[:, c * 128:(c + 1) * 128], b.t[:, c * 128:(c + 1) * 128], cb['ident'].t[:]),
                 reads=[b, cb['ident']], writes=[PB[6]])
        S.op('dve', lambda e: e.tensor_copy(xT.t[:, :, tile_idx * 128:(tile_idx + 1) * 128], pT.rearrange("p (c n) -> p c n", c=8)),
             reads=[PB[6]], writes=[xT])

    def mm8(pj_ap, pj, lhs_fn, rhs_fn, reads):
        for k in range(8):
            l_ap = lhs_fn(k)
            r_ap = rhs_fn(k)
            S.op('pe', lambda e, k=k, l_ap=l_ap, r_ap=r_ap: e.matmul(pj_ap, l_ap, r_ap, start=(k == 0), stop=(k == 7)), reads=reads, writes=[pj])

    def mix_stage(s):
        load_ln(0)
        for tb in range(4):
            tsl = slice(tb * 512, (tb + 1) * 512)
            for cc in range(8):
                csl = slice(cc * 128, (cc + 1) * 128)
                wb = load_w([(w_diff_o[:, csl], 0), (w_gdn_o[:, csl], 128),
                             (w_in[:, O_GTA + cc * 128:O_GTA + (cc + 1) * 128], 256), (w_in[:, O_GTB + cc * 128:O_GTB + (cc + 1) * 128], 384)])
                mm8(PB[0].t[:], PB[0], lambda k, wb=wb: wb.t[:, k, 0:128], lambda k: onT.t[:, k, tsl], [wb, onT])
                mm8(PB[1].t[:], PB[1], lambda k, wb=wb: wb.t[:, k, 128:256], lambda k: gdnT.t[:, k, tsl], [wb, gdnT])
                mm8(PB[2].t[:], PB[2], lambda k, wb=wb: wb.t[:, k, 256:384], lambda k: xT.t[:, k, tsl], [wb, xT])
                mm8(PB[3].t[:], PB[3], lambda k, wb=wb: wb.t[:, k, 384:512], lambda k: xT.t[:, k, tsl], [wb, xT])
                S.op('act', lambda e: e.activation(rt1.t[:], PB[2].t[:], AF.Sigmoid), reads=[PB[2]], writes=[rt1])
                S.op('act', lambda e: e.activation(rt2.t[:], PB[3].t[:], AF.Sigmoid), reads=[PB[3]], writes=[rt2])
                S.op('dve', lambda e: e.tensor_tensor(dbgt.t[:], PB[0].t[:], rt1.t[:], ALU.mult), reads=[PB[0], rt1], writes=[dbgt])
                S.op('dve', lambda e: e.tensor_tensor(rt2.t[:], PB[1].t[:], rt2.t[:], ALU.mult), reads=[PB[1], rt2], writes=[rt2])
                S.op('pool', lambda e, cc=cc: e.tensor_tensor(gated[:, cc, :], dbgt.t[:], rt2.t[:], ALU.add), reads=[dbgt, rt2], writes=[rr_a])
            wm = [load_w([(w_mix_o[:, hf * 512:(hf + 1) * 512], 0)]) for hf in range(2)]
            for tt in range(4):
                t = tb * 4 + tt
                f = xf[0]
                S.dma('sp', f.t[:], x_d[s, t * 128:(t + 1) * 128, :], key=f.name, writes=[f])
                for hf in range(2):
                    pj = PB[4 + hf]
                    mm8(pj.t[:], pj, lambda k, tt=tt: gated[:, k, tt * 128:(tt + 1) * 128], lambda k, hf=hf: wm[hf].t[:, k, :], [rr_a, wm[hf]])
                    S.op('dve', lambda e, hf=hf, pj=pj, f=f: e.scalar_tensor_tensor(lny[:, hf * 512:(hf + 1) * 512], f.t[:, hf * 512:(hf + 1) * 512], ALPHA, pj.t[:], ALU.mult, ALU.add),
                         reads=[f, pj], writes=[ang])
                layer_norm()
                S.dma('sp', x1_d[s * S_LEN + t * 128:s * S_LEN + (t + 1) * 128, :], lno, key='x1st', reads=[ang], writes=[x1res])
                to_xT(t)

    def cross_stage(s):
        load_ln(1)
        pT = PB[6].t[:].bitcast(BF16)
        memT = qT.t[:].rearrange("p (a b) -> p a b", a=8)
        kcT = kT.t[:].rearrange("p (a b) -> p a b", a=8)
        vm = qraw.t[:].rearrange("p (a b) -> p a b", a=2)
        qcT = rr_a.t[:].bitcast(BF16)[:, 0:4096].rearrange("p (a b) -> p a b", a=8)
        ocT = onT.t[:].rearrange("p a b -> p (a b)")[:, 0:4096].rearrange("p (a b) -> p a b", a=8)
        QT, KT = qT, kT
        for mt in range(2):
            f = xf[0]; b = xb[0]
            S.dma('sp', f.t[:], mem_d[s, mt * 128:(mt + 1) * 128, :], key=f.name, writes=[f])
            S.op('act', lambda e, f=f, b=b: e.copy(b.t[:], f.t[:]), reads=[f], writes=[b])
            for c in range(8):
                S.op('pe', lambda e, c=c, b=b: e.transpose(pT[:, c * 128:(c + 1) * 128], b.t[:, c * 128:(c + 1) * 128], cb['ident'].t[:]),
                     reads=[b, cb['ident']], writes=[PB[6]])
            S.op('dve', lambda e, mt=mt: e.tensor_copy(memT[:, :, mt * 128:(mt + 1) * 128], pT.rearrange("p (c n) -> p c n", c=8)), reads=[PB[6]], writes=[QT])
        for hf in range(2):
            wk = load_w([(w_ck[:, hf * 512:(hf + 1) * 512], 0)])
            for c4 in range(4):
                mm8(PB[0].t[:, 0:256], PB[0], lambda k, wk=wk, c4=c4: wk.t[:, k, c4 * 128:(c4 + 1) * 128], lambda k: memT[:, k, :], [wk, QT])
                S.op('act', lambda e, hf=hf, c4=c4: e.copy(kcT[:, hf * 4 + c4, :], PB[0].t[:, 0:256]), reads=[PB[0]], writes=[KT])
        for hf in range(2):
            wv = load_w([(w_cv[:, hf * 512:(hf + 1) * 512], 0)])
            for mt in range(2):
                mm8(PB[1].t[:], PB[1], lambda k, mt=mt: memT[:, k, mt * 128:(mt + 1) * 128], lambda k, wv=wv: wv.t[:, k, :], [wv, QT])
                S.op('act', lambda e, hf=hf, mt=mt: e.copy(vm[:, mt, hf * 512:(hf + 1) * 512], PB[1].t[:]), reads=[PB[1]], writes=[qraw])
        for tb in range(4):
            tsl = slice(tb * 512, (tb + 1) * 512)
            for hf in range(2):
                wq = load_w([(w_cq[:, hf * 512:(hf + 1) * 512], 0)])
                for c4 in range(4):
                    mm8(PB[0].t[:], PB[0], lambda k, wq=wq, c4=c4: wq.t[:, k, c4 * 128:(c4 + 1) * 128], lambda k: xT.t[:, k, tsl], [wq, xT])
                    S.op('act', lambda e, hf=hf, c4=c4: e.copy(qcT[:, hf * 4 + c4, :], PB[0].t[:]), reads=[PB[0]], writes=[rr_a])
            for hh in range(4):
                for mt in range(2):
                    pj = PB[2 + mt]
                    for j in range(2):
                        S.op('pe', lambda e, j=j, mt=mt, hh=hh, pj=pj: e.matmul(pj.t[:], kcT[:, 2 * hh + j, mt * 128:(mt + 1) * 128], qcT[:, 2 * hh + j, :],
                                                                              start=(j == 0), stop=(j == 1)), reads=[KT, rr_a], writes=[pj])
                    S.op('act', lambda e, mt=mt, pj=pj: e.activation(PT[mt].t[:], pj.t[:], AF.Exp, scale=1.0 / 16), reads=[pj], writes=[PT[mt]])
                for mt in range(2):
                    S.op('pe', lambda e, mt=mt: e.matmul(PB[7].t[:], cb['ones'].t[:], PT[mt].t[:], start=(mt == 0), stop=(mt == 1)),
                         reads=[cb['ones'], PT[mt]], writes=[PB[7]])
                S.op('dve', lambda e: e.reciprocal(rt1.t[:], PB[7].t[:]), reads=[PB[7]], writes=[rt1])
                for j in range(2):
                    pj = PB[j]
                    for mt in range(2):
                        S.op('pe', lambda e, j=j, mt=mt, hh=hh, pj=pj: e.matmul(pj.t[:], vm[:, mt, (2 * hh + j) * 128:(2 * hh + j + 1) * 128], PT[mt].t[:],
                                                                              start=(mt == 0), stop=(mt == 1)), reads=[qraw, PT[mt]], writes=[pj])
                    S.op('dve', lambda e, j=j, hh=hh, pj=pj: e.tensor_tensor(ocT[:, 2 * hh + j, :], pj.t[:], rt1.t[:], ALU.mult), reads=[pj, rt1], writes=[onT])
            wo = [load_w([(w_co[:, hf * 512:(hf + 1) * 512], 0)]) for hf in range(2)]
            for tt in range(4):
                t = tb * 4 + tt
                f = xf[0]
                S.dma('sp', f.t[:], x1_d[s * S_LEN + t * 128:s * S_LEN + (t + 1) * 128, :], key=f.name, reads=[x1res], writes=[f])
                for hf in range(2):
                    pj = PB[4 + hf]
                    mm8(pj.t[:], pj, lambda k, tt=tt: ocT[:, k, tt * 128:(tt + 1) * 128], lambda k, hf=hf: wo[hf].t[:, k, :], [onT, wo[hf]])
                    S.op('dve', lambda e, hf=hf, pj=pj, f=f: e.scalar_tensor_tensor(lny[:, hf * 512:(hf + 1) * 512], f.t[:, hf * 512:(hf + 1) * 512], ALPHA, pj.t[:], ALU.mult, ALU.add),
                         reads=[f, pj], writes=[ang])
                layer_norm()
                S.dma('sp', x2_d[s * S_LEN + t * 128:s * S_LEN + (t + 1) * 128, :], lno, key='x2st', reads=[ang], writes=[x2res])

    def moe_stage():
        load_ln(2)
        pT = PB[6].t[:].bitcast(BF16)
        x2f = onT.t[:].rearrange("p a b -> p (a b)").bitcast(F32).rearrange("p (a b) -> p a b", a=8)
        acc = gdnT.t[:].rearrange("p a b -> p (a b)").bitcast(F32).rearrange("p (a b) -> p a b", a=8)
        xflat = xT.t[:].rearrange("p a b -> p (a b)")
        x2T = xflat[:, 0:8192].rearrange("p (a b) -> p a b", a=8)
        actT = xflat[:, 8192:16384].rearrange("p (a b) -> p a b", a=8)
        wr = S.sb("wr", [128, 8, 32], F32)
        S.dma('sp', wr.t[:], w_router.rearrange("(c p) n -> p c n", p=128), key='c_wr', writes=[wr])
        brt = S.sb("brt", [128, 32], F32)
        S.dma('sp', brt.t[:], b_router.broadcast_to([128, 32]), key='c_brt', writes=[brt])
        b1T = sinT
        b1v = sinT.t[:].bitcast(F32)[:, 0:512]
        S.dma('sp', b1v, b1T_d, key='c_b1T', writes=[b1T])
        b2a = cosT
        b2v = cosT.t[:].bitcast(F32)[0:32, :]
        S.dma('sp', b2v, b_e2, key='c_b2a', writes=[b2a])
        gates = S.sb("gates", [128, 8, 32], F32)
        gT = S.sb("gT", [32, 128], F32)
        lg = S.sb("lg", [128, 32], F32)
        ex = S.sb("ex", [128, 32], F32)
        mk = S.sb("mk", [128, 32], F32)
        m8 = S.sb("m8", [128, 8], F32)
        rsm = S.sb("rsm", [128, 4], F32)
        for blk in range(n_seq * 2):
            tok0 = blk * 1024
            for t in range(8):
                rows = slice(tok0 + t * 128, tok0 + (t + 1) * 128)
                S.dma('sp', x2f[:, t, :], x2_d[rows, :], key='x2f', reads=[x2res], writes=[onT])
                b = xb[0]
                S.op('act', lambda e, t=t, b=b: e.copy(b.t[:], x2f[:, t, :]), reads=[onT], writes=[b])
                for c in range(8):
                    S.op('pe', lambda e, c=c, b=b: e.transpose(pT[:, c * 128:(c + 1) * 128], b.t[:, c * 128:(c + 1) * 128], cb['ident'].t[:]),
                         reads=[b, cb['ident']], writes=[PB[6]])
                S.op('dve', lambda e, t=t: e.tensor_copy(x2T[:, :, t * 128:(t + 1) * 128], pT.rearrange("p (c n) -> p c n", c=8)), reads=[PB[6]], writes=[xT])
                f = xf[0]
                for g4 in range(2):
                    for c in range(4):
                        S.op('pe', lambda e, c=c, g4=g4, t=t: e.transpose(PB[7].t[:, c * 128:(c + 1) * 128], x2f[:, t, (g4 * 4 + c) * 128:(g4 * 4 + c + 1) * 128], cf['ident'].t[:]),
                             reads=[onT, cf['ident']], writes=[PB[7]])
                    S.op('act', lambda e, g4=g4, f=f: e.copy(f.t[:, g4 * 512:(g4 + 1) * 512], PB[7].t[:]), reads=[PB[7]], writes=[f])
                for k in range(8):
                    S.op('pe', lambda e, k=k, f=f: e.matmul(PB[2].t[:, 0:32], f.t[:, k * 128:(k + 1) * 128], wr.t[:, k, :], start=(k == 0), stop=(k == 7)),
                         reads=[f, wr], writes=[PB[2]])
                S.op('dve', lambda e: e.tensor_tensor(lg.t[:], PB[2].t[:, 0:32], brt.t[:], ALU.add), reads=[PB[2], brt], writes=[lg])
                S.op('dve', lambda e: e.max(m8.t[:], lg.t[:]), reads=[lg], writes=[m8])
                S.op('dve', lambda e: e.tensor_scalar(mk.t[:], lg.t[:], m8.t[:, 3:4], None, ALU.is_ge), reads=[lg, m8], writes=[mk])
                S.op('dve', lambda e: e.tensor_scalar(rsm.t[:, 0:1], m8.t[:, 0:1], -1.0, None, ALU.mult), reads=[m8], writes=[rsm])
                S.op('act', lambda e: e.activation(ex.t[:], lg.t[:], AF.Exp, bias=rsm.t[:, 0:1], scale=1.0), reads=[lg, rsm], writes=[ex])
                S.op('dve', lambda e: e.tensor_tensor(ex.t[:], ex.t[:], mk.t[:], ALU.mult), reads=[ex, mk], writes=[ex])
                S.op('dve', lambda e: e.reduce_sum(rsm.t[:, 1:2], ex.t[:], axis=AX.X), reads=[ex], writes=[rsm])
                S.op('dve', lambda e: e.reciprocal(rsm.t[:, 2:3], rsm.t[:, 1:2]), reads=[rsm], writes=[rsm])
                S.op('dve', lambda e, t=t: e.tensor_scalar(gates.t[:, t, :], ex.t[:], rsm.t[:, 2:3], None, ALU.mult), reads=[ex, rsm], writes=[gates])
                S.op('pe', lambda e, t=t: e.transpose(PB[3].t[0:32, 0:128], gates.t[:, t, :], cf['ident'].t[:]), reads=[gates, cf['ident']], writes=[PB[3]])
                S.op('act', lambda e: e.copy(gT.t[:], PB[3].t[0:32, 0:128]), reads=[PB[3]], writes=[gT])
                for hf in range(2):
                    pj = PB[4 + hf]
                    S.op('pe', lambda e, hf=hf, pj=pj: e.matmul(pj.t[:], gT.t[:], b2v[:, hf * 512:(hf + 1) * 512], start=True, stop=True), reads=[gT, b2a], writes=[pj])
                    S.op('act', lambda e, hf=hf, pj=pj, t=t: e.copy(acc[:, t, hf * 512:(hf + 1) * 512], pj.t[:]), reads=[pj], writes=[gdnT])
            for ex_i in range(32):
                for pr in range(4):
                    wb = load_w([(w_e1[ex_i][:, pr * 256:(pr + 1) * 256], 0), (w_e1[ex_i][:, 1024 + pr * 256:1024 + (pr + 1) * 256], 256)])
                    for fcl in range(2):
                        fc = pr * 2 + fcl
                        for tbk in range(2):
                            tsl = slice(tbk * 512, (tbk + 1) * 512)
                            mm8(PB[0].t[:], PB[0], lambda k, wb=wb, fcl=fcl: wb.t[:, k, fcl * 128:(fcl + 1) * 128], lambda k, tsl=tsl: x2T[:, k, tsl], [wb, xT])
                            mm8(PB[1].t[:], PB[1], lambda k, wb=wb, fcl=fcl: wb.t[:, k, 256 + fcl * 128:256 + (fcl + 1) * 128], lambda k, tsl=tsl: x2T[:, k, tsl], [wb, xT])
                            cg = ex_i * 16 + fc
                            cu = ex_i * 16 + 8 + fc
                            S.op('dve', lambda e, cg=cg: e.tensor_scalar(rt1.t[:], PB[0].t[:], b1v[:, cg:cg + 1], 7.0, ALU.add, ALU.min), reads=[PB[0], b1T], writes=[rt1])
                            S.op('dve', lambda e, cu=cu: e.tensor_scalar(rt2.t[:], PB[1].t[:], b1v[:, cu:cu + 1], 7.0, ALU.add, ALU.min), reads=[PB[1], b1T], writes=[rt2])
                            S.op('pool', lambda e: e.tensor_scalar(rt2.t[:], rt2.t[:], -7.0, 1.0, ALU.max, ALU.add), reads=[rt2], writes=[rt2])
                            S.op('act', lambda e: e.activation(dbgt.t[:], rt1.t[:], AF.Sigmoid, scale=1.702), reads=[rt1], writes=[dbgt])
                            S.op('pool', lambda e: e.tensor_tensor(rt1.t[:], rt1.t[:], dbgt.t[:], ALU.mult), reads=[rt1, dbgt], writes=[rt1])
                            S.op('pool', lambda e, fc=fc, tsl=tsl: e.tensor_tensor(actT[:, fc, tsl], rt1.t[:], rt2.t[:], ALU.mult), reads=[rt1, rt2], writes=[xT_act])
                w2 = [load_w([(w_e2[ex_i][:, hf * 512:(hf + 1) * 512], 0)]) for hf in range(2)]
                for t in range(8):
                    for hf in range(2):
                        pj = PB[4 + hf]
                        mm8(pj.t[:], pj, lambda k, t=t: actT[:, k, t * 128:(t + 1) * 128], lambda k, hf=hf: w2[hf].t[:, k, :], [xT_act, w2[hf]])
                        S.op('dve', lambda e, t=t, hf=hf, pj=pj, ex_i=ex_i: e.scalar_tensor_tensor(
                            acc[:, t, hf * 512:(hf + 1) * 512], pj.t[:], gates.t[:, t, ex_i:ex_i + 1], acc[:, t, hf * 512:(hf + 1) * 512], ALU.mult, ALU.add),
                            reads=[pj, gates, gdnT], writes=[gdnT])
            for t in range(8):
                S.op('dve', lambda e, t=t: e.scalar_tensor_tensor(lny, x2f[:, t, :], ALPHA, acc[:, t, :], ALU.mult, ALU.add), reads=[onT, gdnT], writes=[ang])
                layer_norm()
                tg = tok0 + t * 128
                S.dma('sp', out_d[tg // S_LEN, tg % S_LEN:tg % S_LEN + 128, :], lno, key='outst', reads=[ang])


    try:
      for s in range(n_seq):
          for t in range(NT):
              f = xf[0]
              b = xb[0]
              S.dma('sp', f.t[:], x_d[s, t * 128:(t + 1) * 128, :], key=f.name, writes=[f])
              S.op('act', lambda e, f=f, b=b: e.copy(b.t[:], f.t[:]), reads=[f], writes=[b])
              pT = PB[6].t[:].bitcast(BF16)
              for c in range(8):
                  S.op('pe', lambda e, c=c, b=b, pT=pT: e.transpose(pT[:, c * 128:(c + 1) * 128], b.t[:, c * 128:(c + 1) * 128], cb['ident'].t[:]),
                       reads=[b, cb['ident']], writes=[PB[6]])
              S.op('dve', lambda e, t=t, pT=pT: e.tensor_copy(xT.t[:, :, t * 128:(t + 1) * 128], pT.rearrange("p (c n) -> p c n", c=8)),
                   reads=[PB[6]], writes=[xT])

          stop('xT', lambda: xT.t[:, 0, 0:512], [xT])
          S.dma('sp', posi.t[:, 0:S_LEN], pos_d[s:s + 1, :].broadcast_to([128, S_LEN]), key='posi', writes=[posi])
          S.op('dve', lambda e: e.tensor_copy(ang.t[:, 0:S_LEN], posi.t[:, 0:S_LEN]), reads=[posi], writes=[ang])
          S.op('dve', lambda e: e.tensor_scalar(ang.t[:, 0:S_LEN], ang.t[:, 0:S_LEN], rcols.t[:, 0:1], None, ALU.mult), reads=[ang, rcols], writes=[ang])
          pf = posi.t[:, 0:S_LEN].bitcast(F32)
          for tab, sh, scol in ((sinT, 0.0, rcols.t[:, 1:2]), (cosT, 0.5 * PI, 1.0)):
              S.op('dve', lambda e, sh=sh: e.tensor_scalar(rr_a.t[:, 0:S_LEN], ang.t[:, 0:S_LEN], sh, None, ALU.add), reads=[ang], writes=[rr_a])
              S.op('dve', lambda e: e.tensor_scalar(posi.t[:, 0:S_LEN], rr_a.t[:, 0:S_LEN], 1.0 / (2 * PI), None, ALU.mult), reads=[rr_a], writes=[posi])
              S.op('dve', lambda e: e.tensor_copy(pf, posi.t[:, 0:S_LEN]), reads=[posi], writes=[posi])
              S.op('dve', lambda e: e.scalar_tensor_tensor(rr_a.t[:, 0:S_LEN], pf, -2 * PI, rr_a.t[:, 0:S_LEN], ALU.mult, ALU.add), reads=[posi, rr_a], writes=[rr_a])
              S.op('dve', lambda e: e.tensor_scalar(pf, rr_a.t[:, 0:S_LEN], PI, None, ALU.is_gt), reads=[rr_a], writes=[posi])
              S.op('dve', lambda e: e.scalar_tensor_tensor(rr_a.t[:, 0:S_LEN], pf, -2 * PI, rr_a.t[:, 0:S_LEN], ALU.mult, ALU.add), reads=[posi, rr_a], writes=[rr_a])
              S.op('dve', lambda e: e.tensor_scalar(rr_a.t[:, 0:S_LEN], rr_a.t[:, 0:S_LEN], -PI, PI, ALU.max, ALU.min), reads=[rr_a], writes=[rr_a])
              S.op('act', lambda e, tab=tab, scol=scol: e.activation(tab.t[:], rr_a.t[:, 0:S_LEN], AF.Sin, scale=scol), reads=[rr_a, rcols], writes=[tab])

          stop('sin', lambda: sinT.t[:, 0:512], [sinT])
          stop('cos', lambda: cosT.t[:, 1536:2048], [cosT])
          for h in range(8):
              wb = load_w([(w_in[:, O_DQ + h * 128:O_DQ + (h + 1) * 128], 0),
                           (w_in[:, O_DK + h * 128:O_DK + (h + 1) * 128], 128),
                           (w_in[:, O_DV + h * 128:O_DV + (h + 1) * 128], 256)])
              for which, dst in ((0, qT), (1, kT)):
                  for tb in range(4):
                      pj = PB[tb % 2]
                      tsl = slice(tb * 512, (tb + 1) * 512)
                      for c in range(8):
                          S.op('pe', lambda e, c=c, pj=pj, tsl=tsl, which=which, wb=wb: e.matmul(
                              pj.t[:], wb.t[:, c, which * 128:(which + 1) * 128], xT.t[:, c, tsl], start=(c == 0), stop=(c == 7)),
                              reads=[wb, xT], writes=[pj])
                      qr = qraw.sub(tb)
                      S.op('act', lambda e, pj=pj, tsl=tsl: e.copy(qraw.t[:, tsl], pj.t[:]), reads=[pj], writes=[qr])
                      S.op('pe', lambda e, tsl=tsl: e.matmul(PB[7].t[:], cb['rope_pm'].t[:], qraw.t[:, tsl], start=True, stop=True),
                           reads=[qr, cb['rope_pm']], writes=[PB[7]])
                      S.op('pool', lambda e, tsl=tsl: e.tensor_tensor(rt1.t[:], qraw.t[:, tsl], cosT.t[:, tsl], ALU.mult),
                           reads=[qr, cosT], writes=[rt1])
                      S.op('dve', lambda e, tsl=tsl: e.tensor_tensor(rt2.t[:], PB[7].t[:], sinT.t[:, tsl], ALU.mult),
                           reads=[PB[7], sinT], writes=[rt2])
                      S.op('dve', lambda e, tsl=tsl, dst=dst: e.tensor_tensor(dst.t[:, tsl], rt1.t[:], rt2.t[:], ALU.add),
                           reads=[rt1, rt2], writes=[dst.sub(tb)])
              stop('qT', lambda: qT.t[:, 0:512], [qT.sub(0)])
              stop('kT', lambda: kT.t[:, 1536:2048], [kT.sub(3)])
              for g in range(4):
                  pj = PB[g % 2]
                  for tt in range(4):
                      t = g * 4 + tt
                      for c in range(8):
                          S.op('pe', lambda e, c=c, pj=pj, t=t, tt=tt, wb=wb: e.matmul(
                              pj.t[:, tt * 128:(tt + 1) * 128], xT.t[:, c, t * 128:(t + 1) * 128], wb.t[:, c, 256:384],
                              start=(c == 0), stop=(c == 7)), reads=[wb, xT], writes=[pj])
                  S.op('act', lambda e, pj=pj, g=g: e.copy(vaug.t[:, g * 4:(g + 1) * 4, 0:128], pj.t[:].rearrange("p (a b) -> p a b", a=4)),
                       reads=[pj], writes=[vaug])
              stop('v', lambda: vaug.t[:, 0:3, :].rearrange('p a b -> p (a b)')[:, 0:396], [vaug])
              for comp in range(2):
                  S.op('pool', lambda e, comp=comp: e.tensor_scalar(kTm[comp].t[:], kT.t[:], cmask.t[:, comp:comp + 1], None, ALU.mult),
                       reads=[kT.sub(0), kT.sub(1), kT.sub(2), kT.sub(3), cmask], writes=[kTm[comp]])
              pctr = 0
              for qb in range(8):
                  qsl = slice(qb * 256, (qb + 1) * 256)
                  nk = 2 * qb + 2
                  for kt in range(nk):
                      sc = PB[2 + (pctr % 2)]
                      pt = PT[pctr % 2]
                      pctr += 1
                      ksl = slice(kt * 128, (kt + 1) * 128)
                      for comp in range(2):
                          S.op('pe', lambda e, comp=comp, sc=sc, ksl=ksl, qsl=qsl: e.matmul(
                              sc.t[:, comp * 256:(comp + 1) * 256], kTm[comp].t[:, ksl], qT.t[:, qsl],
                              start=True, stop=True), reads=[kTm[comp], qT.sub(qb // 2)], writes=[sc])
                      S.op('act', lambda e, sc=sc, pt=pt: e.activation(pt.t[:], sc.t[:], AF.Exp, scale=0.125), reads=[sc], writes=[pt])
                      subs = [0, 1]
                      if kt == 2 * qb:
                          for comp in range(2):
                              S.op('pool', lambda e, pt=pt, comp=comp: e.tensor_tensor(
                                  pt.t[:, comp * 256:comp * 256 + 128], pt.t[:, comp * 256:comp * 256 + 128], cb['u_incl'].t[:], ALU.mult),
                                  reads=[pt, cb['u_incl']], writes=[pt])
                      if kt == 2 * qb + 1:
                          subs = [1]
                          for comp in range(2):
                              S.op('pool', lambda e, pt=pt, comp=comp: e.tensor_tensor(
                                  pt.t[:, comp * 256 + 128:comp * 256 + 256], pt.t[:, comp * 256 + 128:comp * 256 + 256], cb['u_incl'].t[:], ALU.mult),
                                  reads=[pt, cb['u_incl']], writes=[pt])
                      stop('pt0', lambda pt=pt: pt.t[:], [pt])
                      for sub in subs:
                          last = 2 * qb + sub
                          for comp in range(2):
                              oacc = PB[4 + comp]
                              S.op('pe', lambda e, comp=comp, sub=sub, pt=pt, kt=kt, oacc=oacc, last=last: e.matmul(
                                  oacc.t[:, sub * 132:sub * 132 + 129], pt.t[:, comp * 256 + sub * 128:comp * 256 + (sub + 1) * 128],
                                  vaug.t[:, kt, 0:129], start=(kt == 0 and sub == 0), stop=(kt == last), skip_group_check=True),
                                  reads=[pt, vaug], writes=[oacc])
                  stop('o0', lambda: PB[4].t[:, 0:264], [PB[4]])
                  for sub in range(2):
                      t = 2 * qb + sub
                      o1 = PB[4].t[:, sub * 132:sub * 132 + 129]
                      o2 = PB[5].t[:, sub * 132:sub * 132 + 129]
                      S.op('dve', lambda e, o1=o1: e.reciprocal(osm.t[:, 0:1], o1[:, 128:129]), reads=[PB[4]], writes=[osm])
                      S.op('dve', lambda e, o2=o2: e.reciprocal(osm.t[:, 1:2], o2[:, 128:129]), reads=[PB[5]], writes=[osm])
                      S.op('dve', lambda e: e.tensor_tensor(osm.t[:, 2:3], osm.t[:, 1:2], NEG_LAM, ALU.mult), reads=[osm, lam_s], writes=[osm])
                      S.op('act', lambda e, o1=o1: e.activation(otmp.t[:], o1[:, 0:128], AF.Copy, scale=osm.t[:, 0:1]), reads=[PB[4], osm], writes=[otmp])
                      S.op('dve', lambda e, o2=o2: e.scalar_tensor_tensor(ocmb.t[:], o2[:, 0:128], osm.t[:, 2:3], otmp.t[:], ALU.mult, ALU.add),
                           reads=[PB[5], osm, otmp], writes=[ocmb])
                      S.op('act', lambda e: e.activation(ojunk.t[:], ocmb.t[:], AF.Square, accum_out=osm.t[:, 3:4]), reads=[ocmb], writes=[ojunk, osm])
                      S.op('act', lambda e: e.activation(osm.t[:, 4:5], osm.t[:, 3:4], AF.Ln, bias=eps6.t[:, 0:1], scale=1.0 / 128), reads=[osm, eps6], writes=[osm])
                      S.op('act', lambda e: e.activation(osm.t[:, 5:6], osm.t[:, 4:5], AF.Exp, scale=-0.5), reads=[osm], writes=[osm])
                      S.op('dve', lambda e: e.scalar_tensor_tensor(ontok.t[:], ocmb.t[:], osm.t[:, 5:6], gsub.t[:], ALU.mult, ALU.mult),
                           reads=[ocmb, osm, gsub], writes=[ontok])
                      stop('on0', lambda: ontok.t[:], [ontok])
                      pT = PB[6].t[:].bitcast(BF16)
                      S.op('pe', lambda e, pT=pT: e.transpose(pT[:, 0:128], ontok.t[:], cb['ident'].t[:]), reads=[ontok, cb['ident']], writes=[PB[6]])
                      S.op('act', lambda e, pT=pT, h=h, t=t: e.copy(onT.t[:, h, t * 128:(t + 1) * 128], pT[:, 0:128]), reads=[PB[6]], writes=[onT])

          if 'gdn' in stages:
              gdn_stage(s)
          if 'mix' in stages:
              mix_stage(s)
              cross_stage(s)
          if 'ogdn' in dbg and s == 0:
              for h in range(8):
                  for tb in range(4):
                      S.op('dve', lambda e, h=h, tb=tb: e.tensor_copy(dbgt.t[:], gdnT.t[:, h, tb * 512:(tb + 1) * 512]), reads=[gdnT], writes=[dbgt])
                      S.dma('sp', dbg_d['ogdn'][h * 128:(h + 1) * 128, tb * 512:(tb + 1) * 512], dbgt.t[:], key='dbgt', reads=[dbgt])
          if 'ondiff' in dbg and s == 0:
              for h in range(8):
                  for tb in range(4):
                      S.op('dve', lambda e, h=h, tb=tb: e.tensor_copy(dbgt.t[:], onT.t[:, h, tb * 512:(tb + 1) * 512]), reads=[onT], writes=[dbgt])
                      S.dma('sp', dbg_d['ondiff'][h * 128:(h + 1) * 128, tb * 512:(tb + 1) * 512], dbgt.t[:], key='dbgt', reads=[dbgt])

    except StopBuild:
        pass
    if 'mix' in stages:
        moe_stage()
    S.finish()
    return nc


ALL_STAGES = ('diff', 'gdn', 'mix')


def make_maps(inp, n_seq, n_cores):
    consts = make_consts()
    g = lambda k: np.ascontiguousarray(np.asarray(inp[k]))
    shared = {
        "w_in": g('w_in')[0],
        "lamv": np.stack([g('diff_lambda_q1')[0], g('diff_lambda_k1')[0], g('diff_lambda_q2')[0], g('diff_lambda_k2')[0]]),
        "subln_g": g('diff_subln_g'), "w_diff_o": g('w_diff_o')[0],
        "conv_wT": np.ascontiguousarray(g('gdn_conv_w')[0].reshape(4, 24, 128).transpose(2, 1, 0).reshape(128, 96)),
        "gdn_sc": np.stack([g('gdn_A_log')[0], g('gdn_dt_bias')[0]]), "gdn_norm_g": g('gdn_norm_g'),
        "w_gdn_o": g('w_gdn_o')[0], "w_mix_o": g('w_mix_o')[0],
        "lnp": np.stack([g('ln1_g')[0], g('ln1_b')[0], g('ln2_g')[0], g('ln2_b')[0], g('ln3_g')[0], g('ln3_b')[0]]),
        "w_cq": g('w_cq')[0], "w_ck": g('w_ck')[0], "w_cv": g('w_cv')[0], "w_co": g('w_co')[0],
        "w_router": g('w_router')[0], "b_router": g('b_router'),
        "w_e1": g('w_exp_in')[0],
        "b1T": np.ascontiguousarray(g('b_exp_in')[0].reshape(32, 16, 128).transpose(2, 0, 1).reshape(128, 512)),
        "w_e2": g('w_exp_out')[0], "b_e2": g('b_exp_out')[0],
    }
    for n in CONST_NAMES:
        shared["c_" + n] = consts[n]
    shared["c_rope_cols"] = consts['rope_cols']
    x, mem, pos = g('x'), g('mem'), g('positions').astype(np.int32)
    maps = []
    for c in range(n_cores):
        m = dict(shared)
        m["x"] = x[c * n_seq:(c + 1) * n_seq]
        m["mem"] = mem[c * n_seq:(c + 1) * n_seq]
        m["pos"] = pos[c * n_seq:(c + 1) * n_seq]
        maps.append(m)
    return maps


def kernel(**inputs):
    nc = build(n_seq=SEQ_PER_CORE, stages=ALL_STAGES)
    maps = make_maps(inputs, SEQ_PER_CORE, NCORES)
    res = run_bass_kernel_spmd(nc, maps, core_ids=list(range(NCORES)))
    out = np.concatenate([np.asarray(r["out"]) for r in res.results], axis=0)
    return out.astype(np.float32)
```

```python
import math
from contextlib import ExitStack
import numpy as np
import concourse.bass as bass
import concourse.mybir as mybir
from concourse.bass_utils import run_bass_kernel_spmd

F32 = mybir.dt.float32
BF16 = mybir.dt.bfloat16
I32 = mybir.dt.int32
ALU = mybir.AluOpType
AF = mybir.ActivationFunctionType
AX = mybir.AxisListType

ENGS = ['pe', 'act', 'dve', 'pool', 'sp']
ATTACH_WAIT = True
D = 1024
S_LEN = 2048
NT = 16
MEM = 256
NCORES = 8
SEQ_PER_CORE = 4
INW = 9232
O_DQ, O_DK, O_DV, O_GQ, O_GK, O_GV, O_GZ, O_GB, O_GA, O_GTA, O_GTB = 0, 1024, 2048, 3072, 4096, 5120, 6144, 7168, 7176, 7184, 8208
LAMBDA_INIT = 0.2
ALPHA = 2.0 ** 0.25
PI = math.pi


class Res:
    __slots__ = ('name', 't', 'w', 'r', 'subs')

    def __init__(self, name, t=None):
        self.name = name
        self.t = t
        self.w = None
        self.r = {}
        self.subs = {}

    def sub(self, k):
        s = self.subs.get(k)
        if s is None:
            s = Res(f"{self.name}.{k}", self.t)
            self.subs[k] = s
        return s


class Sched:
    def __init__(self, nc):
        self.nc = nc
        self.stack = ExitStack()
        self.ops = {e: [] for e in ENGS}
        self.seen = {e: {} for e in ENGS}
        self.dma_cnt = {}
        self.halt = False

    def sb(self, name, shape, dtype):
        t = self.stack.enter_context(self.nc.sbuf_tensor(name, list(shape), dtype))
        return Res(name, t)

    def ps(self, name, shape, dtype):
        t = self.stack.enter_context(self.nc.psum_tensor(name, list(shape), dtype))
        return Res(name, t)

    def _deps(self, eng, reads, writes):
        deps = {}

        def add(tok):
            if tok is None:
                return
            k = (tok[0], tok[1])
            if deps.get(k, -1) < tok[2]:
                deps[k] = tok[2]
        for r in reads:
            add(r.w)
        for w in writes:
            add(w.w)
            for k, v in w.r.items():
                add((k[0], k[1], v))
        out = []
        seen = self.seen[eng]
        for k, v in deps.items():
            if k[0] == 'e' and k[1] == 'pe' and eng == 'pe':
                continue
            if seen.get(k, -1) >= v:
                continue
            seen[k] = v
            out.append((k[0], k[1], v))
        return out

    def _mark(self, tok, reads, writes):
        k = (tok[0], tok[1])
        for w in writes:
            w.w = tok
            w.r = {}
        for r in reads:
            if r.r.get(k, -1) < tok[2]:
                r.r[k] = tok[2]

    def op(self, eng, fn, reads=(), writes=()):
        if self.halt:
            return
        waits = self._deps(eng, reads, writes)
        idx = len(self.ops[eng])
        self.ops[eng].append([fn, waits, 'c', None, False])
        self._mark(('e', eng, idx), reads, writes)

    def dma(self, q, out, in_, key, reads=(), writes=(), **kw):
        self.dma_fn(q, (lambda e: e.dma_start(out=out, in_=in_, **kw)), key, reads, writes)

    def dma_fn(self, q, fn, key, reads=(), writes=()):
        if self.halt:
            return
        waits = self._deps(q, reads, writes)
        v = self.dma_cnt.get(key, 0) + 16
        self.dma_cnt[key] = v
        self.ops[q].append([fn, waits, 'd', key, False])
        self._mark(('d', key, v), reads, writes)

    def finish(self):
        nc = self.nc
        fin = [('d', key, v) for key, v in self.dma_cnt.items()]
        for e in ENGS:
            for o in self.ops[e]:
                for tok in o[1]:
                    if tok[0] == 'e':
                        self.ops[tok[1]][tok[2]][4] = True
        cum = {}
        for e in ENGS:
            c = 0
            arr = []
            for o in self.ops[e]:
                if o[2] == 'c' and o[4]:
                    c += 1
                arr.append(c)
            cum[e] = arr
        esem = {e: self.stack.enter_context(nc.semaphore(f"s_{e}")) for e in ENGS}
        dsem = {k: self.stack.enter_context(nc.semaphore(f"d_{i}")) for i, k in enumerate(self.dma_cnt)}
        self.n_sems = len(esem) + len(dsem)

        def emit(e, eng):
            for o in self.ops[e]:
                fn, waits, kind, key, marked = o
                wl = [(esem[tok[1]], cum[tok[1]][tok[2]]) if tok[0] == 'e' else (dsem[tok[1]], tok[2]) for tok in waits]
                att = None
                if ATTACH_WAIT and wl:
                    att = wl.pop()
                for sm, vv in wl:
                    eng.wait_ge(sm, vv)
                ins = fn(eng)
                if att is not None:
                    ins._wait_ge(att[0], att[1])
                if kind == 'd':
                    ins.then_inc(dsem[key], 16)
                elif marked:
                    ins.then_inc(esem[e], 1)
            if e == 'sp':
                for tok in fin:
                    eng.wait_ge(dsem[tok[1]], tok[2])

        with nc.Block() as block:
            @block.tensor
            def _(eng):
                emit('pe', eng)

            @block.scalar
            def _(eng):
                emit('act', eng)

            @block.vector
            def _(eng):
                emit('dve', eng)

            @block.gpsimd
            def _(eng):
                emit('pool', eng)

            @block.sync
            def _(eng):
                emit('sp', eng)
        self.stack.close()


def make_consts():
    c = {}
    idx = np.arange(128)
    c['ident'] = np.eye(128, dtype=np.float32)
    c['u_incl'] = (idx[:, None] <= idx[None, :]).astype(np.float32)
    c['u_strict'] = (idx[:, None] < idx[None, :]).astype(np.float32)
    c['l_gt'] = (idx[:, None] > idx[None, :]).astype(np.float32)
    c['ones'] = np.ones((128, 128), np.float32)
    inv_freq = np.power(500000.0, -np.arange(0, 16, 2, dtype=np.float32) / 16).astype(np.float32)
    cols = np.zeros((128, 4), np.float32)
    pm = np.zeros((128, 128), np.float32)
    for p in range(128):
        d = p % 64
        base = p - d
        if d < 8:
            cols[p, 0] = inv_freq[d]
            cols[p, 1] = -1.0
            cols[p, 2] = PI
            pm[base + d + 8, p] = 1.0
        elif d < 16:
            cols[p, 0] = inv_freq[d - 8]
            cols[p, 1] = 1.0
            cols[p, 2] = -PI
            pm[base + d - 8, p] = 1.0
        else:
            cols[p, 1] = 1.0
            cols[p, 2] = -PI
    c['rope_cols'] = cols
    c['rope_pm'] = pm
    return c


CONST_NAMES = ['ident', 'u_incl', 'u_strict', 'l_gt', 'ones', 'rope_pm']


class StopBuild(Exception):
    pass


def build(n_seq=SEQ_PER_CORE, stages=('diff',), dbg=(), stop_at=None):
    nc = bass.Bass("TRN2", target_bir_lowering=False)
    S = Sched(nc)

    def dram_in(name, shape, dt=F32):
        return nc.dram_tensor(name, list(shape), dt, kind="ExternalInput").ap()

    x_d = dram_in("x", [n_seq, S_LEN, D])
    mem_d = dram_in("mem", [n_seq, MEM, D])
    pos_d = dram_in("pos", [n_seq, S_LEN], I32)
    w_in = dram_in("w_in", [D, INW])
    lamv = dram_in("lamv", [4, 64])
    subln_g = dram_in("subln_g", [1, 128])
    w_diff_o = dram_in("w_diff_o", [D, D])
    cdr = {n: dram_in("c_" + n, [128, 128]) for n in CONST_NAMES}
    rope_cols_d = dram_in("c_rope_cols", [128, 4])
    out_d = nc.dram_tensor("out", [n_seq, S_LEN, D], F32, kind="ExternalOutput").ap()
    dbg_d = {}
    if stop_at is not None:
        dbg_d['stop'] = nc.dram_tensor("dbg_stop", [128, 512], F32, kind="ExternalOutput").ap()

    def stop(name, ap_fn, res):
        if stop_at == name:
            S.op('dve', lambda e: e.tensor_copy(dbgt.t[:, 0:ap_fn().shape[-1]], ap_fn()), reads=res, writes=[dbgt])
            S.dma('sp', dbg_d['stop'], dbgt.t[:], key='dbgt', reads=[dbgt])
            S.halt = True
    if 'ogdn' in dbg:
        dbg_d['ogdn'] = nc.dram_tensor("dbg_ogdn", [D, S_LEN], F32, kind="ExternalOutput").ap()
    if 'ondiff' in dbg:
        dbg_d['ondiff'] = nc.dram_tensor("dbg_ondiff", [D, S_LEN], F32, kind="ExternalOutput").ap()

    dbgt = S.sb("dbgt", [128, 512], F32)
    S.op('dve', lambda e: e.memset(dbgt.t[:], 0.0), writes=[dbgt])
    cf = {n: S.sb("cf_" + n, [128, 128], F32) for n in CONST_NAMES}
    cb = {n: S.sb("cb_" + n, [128, 128], BF16) for n in ['ident', 'ones', 'rope_pm', 'u_incl', 'u_strict']}
    for n in CONST_NAMES:
        S.dma('sp', cf[n].t[:], cdr[n], key='c_' + n, writes=[cf[n]])
    for n in cb:
        S.op('dve', lambda e, n=n: e.tensor_copy(cb[n].t[:], cf[n].t[:]), reads=[cf[n]], writes=[cb[n]])
    rcols = S.sb("rcols", [128, 4], F32)
    S.dma('sp', rcols.t[:], rope_cols_d, key='c_rcols', writes=[rcols])

    stop('c0', lambda: cb['rope_pm'].t[:], [cb['rope_pm']])
    lamt = S.sb("lamt", [128, 4, 64], F32)
    S.dma('sp', lamt.t[:], lamv.rearrange("(o a) b -> o a b", o=1).broadcast_to([128, 4, 64]), key='c_lam', writes=[lamt])
    lam_s = S.sb("lam_s", [128, 8], F32)
    lam_j = S.sb("lam_j", [128, 64], F32)
    for i in range(2):
        S.op('dve', lambda e, i=i: e.tensor_tensor(lam_j.t[:], lamt.t[:, 2 * i, :], lamt.t[:, 2 * i + 1, :], ALU.mult),
             reads=[lamt], writes=[lam_j])
        S.op('dve', lambda e, i=i: e.reduce_sum(lam_s.t[:, i:i + 1], lam_j.t[:], axis=AX.X), reads=[lam_j], writes=[lam_s])
    S.op('act', lambda e: e.activation(lam_s.t[:, 2:4], lam_s.t[:, 0:2], AF.Exp), reads=[lam_s], writes=[lam_s])
    S.op('dve', lambda e: e.tensor_tensor(lam_s.t[:, 4:5], lam_s.t[:, 3:4], lam_s.t[:, 2:3], ALU.subtract), reads=[lam_s], writes=[lam_s])
    S.op('dve', lambda e: e.tensor_scalar(lam_s.t[:, 5:6], lam_s.t[:, 4:5], -LAMBDA_INIT, None, ALU.add), reads=[lam_s], writes=[lam_s])
    NEG_LAM = lam_s.t[:, 5:6]
    stop('lam', lambda: lam_s.t[:, 0:8], [lam_s])
    gsub = S.sb("gsub", [128, 128], F32)
    S.dma('sp', gsub.t[:], subln_g.broadcast_to([128, 128]), key='c_gsub', writes=[gsub])
    S.op('dve', lambda e: e.tensor_scalar(gsub.t[:], gsub.t[:], 1.0 - LAMBDA_INIT, None, ALU.mult), reads=[gsub], writes=[gsub])

    stop('gsub', lambda: gsub.t[:], [gsub])
    xT = S.sb("xT", [128, 8, S_LEN], BF16)
    onT = S.sb("onT", [128, 8, S_LEN], BF16)
    xf = [S.sb(f"xf{i}", [128, D], F32) for i in range(1)]
    xb = [S.sb(f"xb{i}", [128, D], BF16) for i in range(1)]
    NWB = 2
    wbuf = [S.sb(f"wbuf{i}", [128, 8, 512], BF16) for i in range(NWB)]
    wctr = [0]
    PB = [S.ps(f"pb{i}", [128, 512], F32) for i in range(8)]

    def load_w(parts):
        wb = wbuf[wctr[0] % NWB]
        wctr[0] += 1
        for ap, off in parts:
            n = ap.shape[1]
            S.dma('pool', wb.t[:, :, off:off + n], ap.rearrange("(c p) n -> p c n", p=128),
                  key=wb.name, writes=[wb])
        return wb

    posi = S.sb("posi", [128, S_LEN + 8], I32)
    ang = S.sb("ang", [128, S_LEN + 8], F32)
    cosT = S.sb("cosT", [128, S_LEN], BF16)
    sinT = S.sb("sinT", [128, S_LEN], BF16)
    rr_a = S.sb("rr_a", [128, S_LEN + 8], F32)
    eps6 = S.sb("eps6", [128, 1], F32)
    S.op('dve', lambda e: e.memset(eps6.t[:], 1e-6), writes=[eps6])

    qraw = S.sb("qraw", [128, S_LEN], BF16)
    qT = S.sb("qT", [128, S_LEN], BF16)
    kT = S.sb("kT", [128, S_LEN], BF16)
    kTm = [S.sb(f"kTm{i}", [128, S_LEN], BF16) for i in range(2)]
    cmask = S.sb("cmask", [128, 2], F32)
    S.op('dve', lambda e: e.memset(cmask.t[:], 0.0), writes=[cmask])
    S.op('dve', lambda e: e.memset(cmask.t[0:64, 0:1], 1.0), writes=[cmask])
    S.op('dve', lambda e: e.memset(cmask.t[64:128, 1:2], 1.0), writes=[cmask])
    vaug = S.sb("vaug", [128, NT, 132], BF16)
    S.op('dve', lambda e: e.memset(vaug.t[:], 1.0), writes=[vaug])
    rt1 = S.sb("rt1", [128, 512], F32)
    rt2 = S.sb("rt2", [128, 512], F32)
    PT = [S.sb(f"PT{i}", [128, 512], BF16) for i in range(2)]
    otmp = S.sb("otmp", [128, 128], F32)
    ocmb = S.sb("ocmb", [128, 128], F32)
    ojunk = S.sb("ojunk", [128, 128], F32)
    ontok = S.sb("ontok", [128, 128], BF16)
    osm = S.sb("osm", [128, 8], F32)

    def bview(res, dt, shape_str=None, **kw):
        ap = res.t[:]
        if len(ap.shape) == 3:
            ap = ap.rearrange("p a b -> p (a b)")
        ap = ap.bitcast(dt)
        if shape_str:
            ap = ap.rearrange(shape_str, **kw)
        return ap

    if 'gdn' in stages:
        conv_wT = dram_in("conv_wT", [128, 96])
        gsc_d = dram_in("gdn_sc", [2, 8])
        gnorm_d = dram_in("gdn_norm_g", [1, 128])
        gdnT = S.sb("gdnT", [128, 8, S_LEN], BF16)
        cw = S.sb("cw", [128, 96], F32)
        S.dma('sp', cw.t[:], conv_wT, key='c_cw', writes=[cw])
        gsc = S.sb("gsc", [128, 2, 8], F32)
        S.dma('sp', gsc.t[:], gsc_d.rearrange("(o a) b -> o a b", o=1).broadcast_to([128, 2, 8]), key='c_gsc', writes=[gsc])
        negA = S.sb("negA", [128, 8], F32)
        S.op('act', lambda e: e.activation(negA.t[:], gsc.t[:, 0, :], AF.Exp), reads=[gsc], writes=[negA])
        S.op('dve', lambda e: e.tensor_scalar(negA.t[:], negA.t[:], -1.0, None, ALU.mult), reads=[negA], writes=[negA])
        gnorm = S.sb("gnorm", [128, 128], F32)
        S.dma('sp', gnorm.t[:], gnorm_d.broadcast_to([128, 128]), key='c_gnorm', writes=[gnorm])
        one1 = S.sb("one1", [128, 1], F32)
        S.op('dve', lambda e: e.memset(one1.t[:], 1.0), writes=[one1])
        sc_ba = S.sb("sc_ba", [128, NT, 16], F32)
        sc_beta = S.sb("sc_beta", [128, NT, 8], F32)
        sc_nbeta = S.sb("sc_nbeta", [128, NT, 8], F32)
        sc_g = S.sb("sc_g", [128, NT, 8], F32)
        sc_eG = S.sb("sc_eG", [128, NT, 8], F32)
        sc_neG = S.sb("sc_neG", [128, NT, 8], F32)
        sc_dk = S.sb("sc_dk", [128, NT, 8], F32)
        sc_gl = S.sb("sc_gl", [128, NT, 8], F32)
        sc_tmp = S.sb("sc_tmp", [128, NT, 8], F32)
        Gm = S.sb("Gm", [128, 128], F32)
        dec = S.sb("dec", [128, 128], F32)
        decI = S.sb("decI", [128, 128], BF16)
        decS = S.sb("decS", [128, 128], BF16)
        Yb = [S.sb(f"Yb{i}", [128, 128], BF16) for i in range(2)]
        Zb = [S.sb(f"Zb{i}", [128, 128], BF16) for i in range(2)]
        Tb = [S.sb(f"Tb{i}", [128, 128], BF16) for i in range(2)]
        Ttall = S.sb("Ttall", [128, 1, 128], BF16)
        QKDall = S.sb("QKDall", [128, 1, 128], BF16)
        zs = S.sb("zs", [128, 1, 128], BF16)
        st_f = S.sb("st_f", [128, 128], F32)
        st_b = S.sb("st_b", [128, 128], BF16)
        Rt = S.sb("Rt", [128, 128], BF16)
        vnew = S.sb("vnew", [128, 128], BF16)
        gtmp = S.sb("gtmp", [128, 128], F32)
        go = S.sb("go", [128, 128], F32)
        gog = S.sb("gog", [128, 128], F32)
        gob = S.sb("gob", [128, 128], BF16)
        gsm = S.sb("gsm", [128, 8], F32)
        RW = S_LEN + 8
        raws = [(ang, 0), (ang, 1), (posi, 0)]
        cacc = rr_a
        sil = rr_a
        qnT, knT, vsT, sqb = qT, kT, qraw, kTm[0]
        kdec = kTm[1]
        vtok = vaug

    def gdn_stage(s):
        pT = PB[6].t[:].bitcast(BF16)
        wba = load_w([(w_in[:, O_GB:O_GB + 16], 0)])
        for t in range(NT):
            for c in range(8):
                S.op('pe', lambda e, c=c, t=t: e.matmul(PB[7].t[:, t * 16:(t + 1) * 16], xT.t[:, c, t * 128:(t + 1) * 128], wba.t[:, c, 0:16],
                                                        start=(c == 0), stop=(c == 7)), reads=[wba, xT], writes=[PB[7]])
        S.op('act', lambda e: e.copy(sc_ba.t[:], PB[7].t[:, 0:256].rearrange("p (a b) -> p a b", a=NT)), reads=[PB[7]], writes=[sc_ba])
        S.op('act', lambda e: e.activation(sc_tmp.t[:], sc_ba.t[:, :, 0:8], AF.Exp, scale=-1.0), reads=[sc_ba], writes=[sc_tmp])
        S.op('dve', lambda e: e.tensor_scalar(sc_tmp.t[:], sc_tmp.t[:], 1.0, None, ALU.add), reads=[sc_tmp], writes=[sc_tmp])
        S.op('dve', lambda e: e.reciprocal(sc_beta.t[:], sc_tmp.t[:]), reads=[sc_tmp], writes=[sc_beta])
        S.op('dve', lambda e: e.tensor_scalar(sc_nbeta.t[:], sc_beta.t[:], -1.0, None, ALU.mult), reads=[sc_beta], writes=[sc_nbeta])
        S.op('dve', lambda e: e.tensor_tensor(sc_tmp.t[:], sc_ba.t[:, :, 8:16], gsc.t[:, 1:2, :].broadcast_to([128, NT, 8]), ALU.add),
             reads=[sc_ba, gsc], writes=[sc_tmp])
        S.op('act', lambda e: e.activation(sc_tmp.t[:], sc_tmp.t[:], AF.Exp), reads=[sc_tmp], writes=[sc_tmp])
        S.op('act', lambda e: e.activation(sc_tmp.t[:], sc_tmp.t[:], AF.Ln, bias=one1.t[:, 0:1], scale=1.0), reads=[sc_tmp, one1], writes=[sc_tmp])
        S.op('dve', lambda e: e.tensor_tensor(sc_g.t[:], sc_tmp.t[:], negA.t[:].unsqueeze(1).broadcast_to([128, NT, 8]), ALU.mult),
             reads=[sc_tmp, negA], writes=[sc_g])
        gflat = sc_g.t[:].rearrange("p a b -> p (a b)")
        S.op('pe', lambda e: e.matmul(PB[7].t[:, 0:128], cf['u_incl'].t[:], gflat, start=True, stop=True), reads=[cf['u_incl'], sc_g], writes=[PB[7]])
        S.op('pe', lambda e: e.matmul(PB[7].t[:, 128:256], cf['ones'].t[:], gflat, start=True, stop=True), reads=[cf['ones'], sc_g], writes=[PB[7]])
        Gps = PB[7].t[:, 0:128].rearrange("p (a b) -> p a b", a=NT)
        GLps = PB[7].t[:, 128:256].rearrange("p (a b) -> p a b", a=NT)
        S.op('act', lambda e: e.activation(sc_eG.t[:], Gps, AF.Exp), reads=[PB[7]], writes=[sc_eG])
        S.op('dve', lambda e: e.tensor_scalar(sc_neG.t[:], sc_eG.t[:], -1.0, None, ALU.mult), reads=[sc_eG], writes=[sc_neG])
        S.op('act', lambda e: e.activation(sc_gl.t[:], GLps, AF.Exp), reads=[PB[7]], writes=[sc_gl])
        S.op('act', lambda e: e.copy(sc_tmp.t[:], Gps), reads=[PB[7]], writes=[sc_tmp])
        S.op('dve', lambda e: e.tensor_tensor(sc_tmp.t[:], GLps, sc_tmp.t[:], ALU.subtract), reads=[PB[7], sc_tmp], writes=[sc_tmp])
        S.op('act', lambda e: e.activation(sc_dk.t[:], sc_tmp.t[:], AF.Exp), reads=[sc_tmp], writes=[sc_dk])
        for r, hf in raws:
            S.op('pool', lambda e, r=r, hf=hf: e.memset(r.t[:].bitcast(BF16)[:, hf * RW:hf * RW + 8], 0.0), writes=[r])

        for h in range(8):
            wb = load_w([(w_in[:, O_GQ + h * 128:O_GQ + (h + 1) * 128], 0), (w_in[:, O_GK + h * 128:O_GK + (h + 1) * 128], 128),
                         (w_in[:, O_GV + h * 128:O_GV + (h + 1) * 128], 256), (w_in[:, O_GZ + h * 128:O_GZ + (h + 1) * 128], 384)])
            for j in range(3):
                raw, hf = raws[j]
                rv = raw.t[:].bitcast(BF16)[:, hf * RW:(hf + 1) * RW]
                for tb in range(4):
                    pj = PB[tb % 2]
                    tsl = slice(tb * 512, (tb + 1) * 512)
                    for c in range(8):
                        S.op('pe', lambda e, c=c, pj=pj, tsl=tsl, j=j, wb=wb: e.matmul(
                            pj.t[:], wb.t[:, c, j * 128:(j + 1) * 128], xT.t[:, c, tsl], start=(c == 0), stop=(c == 7)),
                            reads=[wb, xT], writes=[pj])
                    S.op('act', lambda e, pj=pj, tb=tb, rv=rv: e.copy(rv[:, 3 + tb * 512:3 + (tb + 1) * 512], pj.t[:]), reads=[pj], writes=[raw])
                cwc = lambda w, j=j, h=h: cw.t[:, (j * 8 + h) * 4 + w:(j * 8 + h) * 4 + w + 1]
                S.op('dve', lambda e, rv=rv, cwc=cwc: e.tensor_scalar(cacc.t[:, 0:S_LEN], rv[:, 3:3 + S_LEN], cwc(3), None, ALU.mult), reads=[raw, cw], writes=[cacc])
                for w_, eng in ((2, 'dve'), (1, 'dve'), (0, 'dve')):
                    S.op(eng, lambda e, rv=rv, cwc=cwc, w_=w_: e.scalar_tensor_tensor(cacc.t[:, 0:S_LEN], rv[:, w_:w_ + S_LEN], cwc(w_), cacc.t[:, 0:S_LEN], ALU.mult, ALU.add),
                         reads=[raw, cw, cacc], writes=[cacc])
                if j == 2:
                    S.op('act', lambda e: e.activation(vsT.t[:], cacc.t[:, 0:S_LEN], AF.Silu), reads=[cacc], writes=[vsT.sub(0), vsT.sub(1), vsT.sub(2), vsT.sub(3)])
                else:
                    dstT = qnT if j == 0 else knT
                    qscale = (128.0 ** -0.5) if j == 0 else 1.0
                    S.op('act', lambda e: e.activation(sil.t[:, 0:S_LEN], cacc.t[:, 0:S_LEN], AF.Silu), reads=[cacc], writes=[sil])
                    S.op('act', lambda e: e.activation(sqb.t[:], sil.t[:, 0:S_LEN], AF.Square), reads=[sil], writes=[sqb])
                    for tb in range(4):
                        tsl = slice(tb * 512, (tb + 1) * 512)
                        S.op('pe', lambda e, tsl=tsl: e.matmul(PB[7].t[:], cb['ones'].t[:], sqb.t[:, tsl], start=True, stop=True),
                             reads=[cb['ones'], sqb], writes=[PB[7]])
                        S.op('act', lambda e: e.activation(rt1.t[:], PB[7].t[:], AF.Ln, bias=eps6.t[:, 0:1], scale=1.0), reads=[PB[7], eps6], writes=[rt1])
                        S.op('act', lambda e: e.activation(rt1.t[:], rt1.t[:], AF.Exp, scale=-0.5), reads=[rt1], writes=[rt1])
                        S.op('dve', lambda e, tsl=tsl, dstT=dstT, qscale=qscale: e.scalar_tensor_tensor(
                            dstT.t[:, tsl], sil.t[:, tsl], qscale, rt1.t[:], ALU.mult, ALU.mult), reads=[sil, rt1], writes=[dstT.sub(tb)])
            for t in range(NT):
                tsl = slice(t * 128, (t + 1) * 128)
                S.op('pe', lambda e, tsl=tsl: e.transpose(pT[:, 0:128], vsT.t[:, tsl], cb['ident'].t[:]), reads=[vsT.sub(t // 4), cb['ident']], writes=[PB[6]])
                S.op('act', lambda e, t=t: e.copy(vtok.t[:, t, 0:128], pT[:, 0:128]), reads=[PB[6]], writes=[vtok])
                S.op('pe', lambda e, tsl=tsl: e.transpose(pT[:, 128:256], knT.t[:, tsl], cb['ident'].t[:]), reads=[knT.sub(t // 4), cb['ident']], writes=[PB[6]])
                S.op('act', lambda e, t=t, h=h, tsl=tsl: e.activation(kdec.t[:, tsl], pT[:, 128:256], AF.Copy, scale=sc_dk.t[:, t, h:h + 1]),
                     reads=[PB[6], sc_dk], writes=[kdec])
            S.op('dve', lambda e: e.memset(st_f.t[:], 0.0), writes=[st_f])
            S.op('dve', lambda e: e.memset(st_b.t[:], 0.0), writes=[st_b])
            for c in range(NT):
                tsl = slice(c * 128, (c + 1) * 128)
                S.op('dve', lambda e, c=c, h=h: e.tensor_scalar(Gm.t[:], cf['u_incl'].t[:], sc_g.t[:, c, h:h + 1], None, ALU.mult),
                     reads=[cf['u_incl'], sc_g], writes=[Gm])
                S.op('pe', lambda e: e.matmul(PB[0].t[:, 0:128], cf['l_gt'].t[:], Gm.t[:], start=True, stop=True), reads=[cf['l_gt'], Gm], writes=[PB[0]])
                S.op('act', lambda e: e.activation(dec.t[:], PB[0].t[:, 0:128], AF.Exp), reads=[PB[0]], writes=[dec])
                S.op('pool', lambda e: e.tensor_tensor(decI.t[:], dec.t[:], cf['u_incl'].t[:], ALU.mult), reads=[dec, cf['u_incl']], writes=[decI])
                S.op('pool', lambda e: e.tensor_tensor(decS.t[:], dec.t[:], cf['u_strict'].t[:], ALU.mult), reads=[dec, cf['u_strict']], writes=[decS])
                S.op('pe', lambda e, tsl=tsl: e.matmul(PB[1].t[:, 0:128], knT.t[:, tsl], knT.t[:, tsl], start=True, stop=True), reads=[knT.sub(c // 4)], writes=[PB[1]])
                S.op('pe', lambda e, tsl=tsl: e.matmul(PB[1].t[:, 128:256], knT.t[:, tsl], qnT.t[:, tsl], start=True, stop=True),
                     reads=[knT.sub(c // 4), qnT.sub(c // 4)], writes=[PB[1]])
                Y, Z, T = Yb[0], Zb[0], Tb[0]
                S.op('dve', lambda e, c=c, h=h, Y=Y: e.scalar_tensor_tensor(Y.t[:], PB[1].t[:, 0:128], sc_nbeta.t[:, c, h:h + 1], decS.t[:], ALU.mult, ALU.mult),
                     reads=[PB[1], sc_nbeta, decS], writes=[Y])
                S.op('dve', lambda e, c=c: e.tensor_tensor(QKDall.t[:, 0, :], PB[1].t[:, 128:256], decI.t[:], ALU.mult), reads=[PB[1], decI], writes=[QKDall])
                S.op('pe', lambda e, Y=Y: e.transpose(pT[:, 0:128], Y.t[:], cb['ident'].t[:]), reads=[Y, cb['ident']], writes=[PB[6]])
                S.op('act', lambda e, Z=Z: e.copy(Z.t[:], pT[:, 0:128]), reads=[PB[6]], writes=[Z])
                S.op('pool', lambda e, Y=Y, T=T: e.tensor_tensor(T.t[:], Y.t[:], cb['ident'].t[:], ALU.add), reads=[Y, cb['ident']], writes=[T])
                cur = 0
                for n in range(1, 7):
                    Y, Z, T = Yb[cur], Zb[cur], Tb[cur]
                    Y2, Z2, T2 = Yb[1 - cur], Zb[1 - cur], Tb[1 - cur]
                    S.op('pe', lambda e, Y=Y, Z=Z: e.matmul(PB[2].t[:, 0:128], Y.t[:], Z.t[:], start=True, stop=True), reads=[Y, Z], writes=[PB[2]])
                    S.op('act', lambda e, Z2=Z2: e.copy(Z2.t[:], PB[2].t[:, 0:128]), reads=[PB[2]], writes=[Z2])
                    if n < 6:
                        S.op('pe', lambda e, Y=Y, Z=Z: e.matmul(PB[3].t[:, 0:128], Z.t[:], Y.t[:], start=True, stop=True), reads=[Y, Z], writes=[PB[3]])
                        S.op('dve', lambda e, Y2=Y2: e.tensor_copy(Y2.t[:], PB[3].t[:, 0:128]), reads=[PB[3]], writes=[Y2])
                    S.op('pe', lambda e, Z2=Z2, T=T: e.matmul(PB[0].t[:, 0:128], Z2.t[:], T.t[:], start=True, stop=True), reads=[Z2, T], writes=[PB[0]])
                    if n < 6:
                        S.op('dve', lambda e, T=T, T2=T2: e.tensor_tensor(T2.t[:], PB[0].t[:, 0:128], T.t[:], ALU.add), reads=[PB[0], T], writes=[T2])
                    else:
                        S.op('dve', lambda e, T=T, c=c: e.tensor_tensor(Ttall.t[:, 0, :], PB[0].t[:, 0:128], T.t[:], ALU.add), reads=[PB[0], T], writes=[Ttall])
                    cur = 1 - cur
                for kc in range(8):
                    S.op('pe', lambda e, kc=kc, tsl=tsl, wb=wb: e.matmul(PB[7].t[:, 0:128], xT.t[:, kc, tsl], wb.t[:, kc, 384:512], start=(kc == 0), stop=(kc == 7)),
                         reads=[wb, xT], writes=[PB[7]])
                S.op('act', lambda e: e.activation(zs.t[:, 0, :], PB[7].t[:, 0:128], AF.Silu), reads=[PB[7]], writes=[zs])
                S.op('pe', lambda e, tsl=tsl: e.matmul(PB[2].t[:, 0:128], knT.t[:, tsl], st_b.t[:], start=True, stop=True), reads=[knT.sub(c // 4), st_b], writes=[PB[2]])
                S.op('dve', lambda e, c=c, h=h: e.scalar_tensor_tensor(Rt.t[:], PB[2].t[:, 0:128], sc_neG.t[:, c, h:h + 1], vtok.t[:, c, 0:128], ALU.mult, ALU.add),
                     reads=[PB[2], sc_neG, vtok], writes=[Rt])
                S.op('pe', lambda e, c=c: e.matmul(PB[3].t[:, 0:128], Ttall.t[:, 0, :], Rt.t[:], start=True, stop=True), reads=[Ttall, Rt], writes=[PB[3]])
                S.op('act', lambda e, c=c, h=h: e.activation(vnew.t[:], PB[3].t[:, 0:128], AF.Copy, scale=sc_beta.t[:, c, h:h + 1]), reads=[PB[3], sc_beta], writes=[vnew])
                S.op('pe', lambda e, tsl=tsl: e.matmul(PB[0].t[:, 0:128], qnT.t[:, tsl], st_b.t[:], start=True, stop=True), reads=[qnT.sub(c // 4), st_b], writes=[PB[0]])
                S.op('pe', lambda e, c=c: e.matmul(PB[1].t[:, 0:128], QKDall.t[:, 0, :], vnew.t[:], start=True, stop=True), reads=[QKDall, vnew], writes=[PB[1]])
                S.op('act', lambda e, c=c, h=h: e.activation(gtmp.t[:], PB[0].t[:, 0:128], AF.Copy, scale=sc_eG.t[:, c, h:h + 1]), reads=[PB[0], sc_eG], writes=[gtmp])
                S.op('dve', lambda e: e.tensor_tensor(go.t[:], PB[1].t[:, 0:128], gtmp.t[:], ALU.add), reads=[PB[1], gtmp], writes=[go])
                S.op('pe', lambda e, tsl=tsl: e.matmul(PB[2].t[:, 128:256], kdec.t[:, tsl], vnew.t[:], start=True, stop=True), reads=[kdec, vnew], writes=[PB[2]])
                S.op('dve', lambda e, c=c, h=h: e.scalar_tensor_tensor(st_f.t[:], st_f.t[:], sc_gl.t[:, c, h:h + 1], PB[2].t[:, 128:256], ALU.mult, ALU.add),
                     reads=[st_f, sc_gl, PB[2]], writes=[st_f])
                S.op('pool', lambda e: e.tensor_copy(st_b.t[:], st_f.t[:]), reads=[st_f], writes=[st_b])
                S.op('act', lambda e: e.activation(gog.t[:], go.t[:], AF.Square, accum_out=gsm.t[:, 0:1]), reads=[go], writes=[gog, gsm])
                S.op('act', lambda e: e.activation(gsm.t[:, 1:2], gsm.t[:, 0:1], AF.Ln, bias=eps6.t[:, 0:1], scale=1.0 / 128), reads=[gsm, eps6], writes=[gsm])
                S.op('act', lambda e: e.activation(gsm.t[:, 2:3], gsm.t[:, 1:2], AF.Exp, scale=-0.5), reads=[gsm], writes=[gsm])
                S.op('dve', lambda e: e.scalar_tensor_tensor(gog.t[:], go.t[:], gsm.t[:, 2:3], gnorm.t[:], ALU.mult, ALU.mult), reads=[go, gsm, gnorm], writes=[gog])
                S.op('pool', lambda e, c=c: e.tensor_tensor(gob.t[:], gog.t[:], zs.t[:, 0, :], ALU.mult), reads=[gog, zs], writes=[gob])
                S.op('pe', lambda e: e.transpose(pT[:, 256:384], gob.t[:], cb['ident'].t[:]), reads=[gob, cb['ident']], writes=[PB[6]])
                S.op('act', lambda e, h=h, tsl=tsl: e.copy(gdnT.t[:, h, tsl], pT[:, 256:384]), reads=[PB[6]], writes=[gdnT])


    x1res = Res('x1res'); x2res = Res('x2res'); xT_act = Res('xT_act')
    if 'mix' in stages:
        w_gdn_o = dram_in("w_gdn_o", [D, D]); w_mix_o = dram_in("w_mix_o", [D, D])
        lnp_d = dram_in("lnp", [6, D])
        w_cq = dram_in("w_cq", [D, D]); w_ck = dram_in("w_ck", [D, D]); w_cv = dram_in("w_cv", [D, D]); w_co = dram_in("w_co", [D, D])
        w_router = dram_in("w_router", [D, 32]); b_router = dram_in("b_router", [1, 32])
        w_e1 = dram_in("w_e1", [32, D, 2048]); b1T_d = dram_in("b1T", [128, 512])
        w_e2 = dram_in("w_e2", [32, D, D]); b_e2 = dram_in("b_e2", [32, D])
        iota_d = dram_in("iota192", [128, 192])
        x1_d = nc.dram_tensor("x1_scr", [n_seq * S_LEN, D], F32, kind=("ExternalOutput" if "x12" in dbg else "Internal")).ap()
        x2_d = nc.dram_tensor("x2_scr", [n_seq * S_LEN, D], F32, kind=("ExternalOutput" if "x12" in dbg else "Internal")).ap()
        lsm = S.sb("lsm", [128, 8], F32)
        eps5 = S.sb("eps5", [128, 1], F32)
        S.op('dve', lambda e: e.memset(eps5.t[:], 1e-5), writes=[eps5])
        gated = rr_a.t[:].bitcast(BF16)[:, 0:4096].rearrange("p (a b) -> p a b", a=8)
        lny = ang.t[:, 0:1024]
        lno = ang.t[:, 1024:2048]
        pfv = posi.t[:].bitcast(F32)
        lng = pfv[:, 0:1024]
        lnb = pfv[:, 1024:2048]

    def load_ln(i):
        S.dma('sp', lng, lnp_d[2 * i:2 * i + 1, :].broadcast_to([128, D]), key='lnp', writes=[posi])
        S.dma('sp', lnb, lnp_d[2 * i + 1:2 * i + 2, :].broadcast_to([128, D]), key='lnp', writes=[posi])

    def layer_norm():
        S.op('act', lambda e: e.activation(lno, lny, AF.Copy, accum_out=lsm.t[:, 0:1]), reads=[ang], writes=[ang, lsm])
        S.op('dve', lambda e: e.tensor_scalar(lsm.t[:, 1:2], lsm.t[:, 0:1], -1.0 / D, None, ALU.mult), reads=[lsm], writes=[lsm])
        S.op('dve', lambda e: e.tensor_scalar(lny, lny, lsm.t[:, 1:2], None, ALU.add), reads=[ang, lsm], writes=[ang])
        S.op('act', lambda e: e.activation(lno, lny, AF.Square, accum_out=lsm.t[:, 2:3]), reads=[ang], writes=[ang, lsm])
        S.op('act', lambda e: e.activation(lsm.t[:, 3:4], lsm.t[:, 2:3], AF.Ln, bias=eps5.t[:, 0:1], scale=1.0 / D), reads=[lsm, eps5], writes=[lsm])
        S.op('act', lambda e: e.activation(lsm.t[:, 4:5], lsm.t[:, 3:4], AF.Exp, scale=-0.5), reads=[lsm], writes=[lsm])
        S.op('dve', lambda e: e.scalar_tensor_tensor(lno, lny, lsm.t[:, 4:5], lng, ALU.mult, ALU.mult), reads=[ang, lsm, posi], writes=[ang])
        S.op('pool', lambda e: e.tensor_tensor(lno, lno, lnb, ALU.add), reads=[ang, posi], writes=[ang])

    def to_xT(tile_idx):
        pT = PB[6].t[:].bitcast(BF16)
        b = xb[0]
        S.op('act', lambda e: e.copy(b.t[:], lno), reads=[ang], writes=[b])
        for c in range(8):
            S.op('pe', lambda e, c=c: e.transpose(pT[:, c * 128:(c + 1) * 128], b.t[:, c * 128:(c + 1) * 128], cb['ident'].t[:]),
                 reads=[b, cb['ident']], writes=[PB[6]])
        S.op('dve', lambda e: e.tensor_copy(xT.t[:, :, tile_idx * 128:(tile_idx + 1) * 128], pT.rearrange("p (c n) -> p c n", c=8)),
             reads=[PB[6]], writes=[xT])

    def mm8(pj_ap, pj, lhs_fn, rhs_fn, reads):
        for k in range(8):
            l_ap = lhs_fn(k)
            r_ap = rhs_fn(k)
            S.op('pe', lambda e, k=k, l_ap=l_ap, r_ap=r_ap: e.matmul(pj_ap, l_ap, r_ap, start=(k == 0), stop=(k == 7)), reads=reads, writes=[pj])

    def mix_stage(s):
        load_ln(0)
        for tb in range(4):
            tsl = slice(tb * 512, (tb + 1) * 512)
            for cc in range(8):
                csl = slice(cc * 128, (cc + 1) * 128)
                wb = load_w([(w_diff_o[:, csl], 0), (w_gdn_o[:, csl], 128),
                             (w_in[:, O_GTA + cc * 128:O_GTA + (cc + 1) * 128], 256), (w_in[:, O_GTB + cc * 128:O_GTB + (cc + 1) * 128], 384)])
                mm8(PB[0].t[:], PB[0], lambda k, wb=wb: wb.t[:, k, 0:128], lambda k: onT.t[:, k, tsl], [wb, onT])
                mm8(PB[1].t[:], PB[1], lambda k, wb=wb: wb.t[:, k, 128:256], lambda k: gdnT.t[:, k, tsl], [wb, gdnT])
                mm8(PB[2].t[:], PB[2], lambda k, wb=wb: wb.t[:, k, 256:384], lambda k: xT.t[:, k, tsl], [wb, xT])
                mm8(PB[3].t[:], PB[3], lambda k, wb=wb: wb.t[:, k, 384:512], lambda k: xT.t[:, k, tsl], [wb, xT])
                S.op('act', lambda e: e.activation(rt1.t[:], PB[2].t[:], AF.Sigmoid), reads=[PB[2]], writes=[rt1])
                S.op('act', lambda e: e.activation(rt2.t[:], PB[3].t[:], AF.Sigmoid), reads=[PB[3]], writes=[rt2])
                S.op('dve', lambda e: e.tensor_tensor(dbgt.t[:], PB[0].t[:], rt1.t[:], ALU.mult), reads=[PB[0], rt1], writes=[dbgt])
                S.op('dve', lambda e: e.tensor_tensor(rt2.t[:], PB[1].t[:], rt2.t[:], ALU.mult), reads=[PB[1], rt2], writes=[rt2])
                S.op('pool', lambda e, cc=cc: e.tensor_tensor(gated[:, cc, :], dbgt.t[:], rt2.t[:], ALU.add), reads=[dbgt, rt2], writes=[rr_a])
            wm = [load_w([(w_mix_o[:, hf * 512:(hf + 1) * 512], 0)]) for hf in range(2)]
            for tt in range(4):
                t = tb * 4 + tt
                f = xf[0]
                S.dma('sp', f.t[:], x_d[s, t * 128:(t + 1) * 128, :], key=f.name, writes=[f])
                for hf in range(2):
                    pj = PB[4 + hf]
                    mm8(pj.t[:], pj, lambda k, tt=tt: gated[:, k, tt * 128:(tt + 1) * 128], lambda k, hf=hf: wm[hf].t[:, k, :], [rr_a, wm[hf]])
                    S.op('dve', lambda e, hf=hf, pj=pj, f=f: e.scalar_tensor_tensor(lny[:, hf * 512:(hf + 1) * 512], f.t[:, hf * 512:(hf + 1) * 512], ALPHA, pj.t[:], ALU.mult, ALU.add),
                         reads=[f, pj], writes=[ang])
                layer_norm()
                S.dma('sp', x1_d[s * S_LEN + t * 128:s * S_LEN + (t + 1) * 128, :], lno, key='x1st', reads=[ang], writes=[x1res])
                to_xT(t)

    def cross_stage(s):
        load_ln(1)
        pT = PB[6].t[:].bitcast(BF16)
        memT = qT.t[:].rearrange("p (a b) -> p a b", a=8)
        kcT = kT.t[:].rearrange("p (a b) -> p a b", a=8)
        vm = qraw.t[:].rearrange("p (a b) -> p a b", a=2)
        qcT = rr_a.t[:].bitcast(BF16)[:, 0:4096].rearrange("p (a b) -> p a b", a=8)
        ocT = onT.t[:].rearrange("p a b -> p (a b)")[:, 0:4096].rearrange("p (a b) -> p a b", a=8)
        QT, KT = qT, kT
        for mt in range(2):
            f = xf[0]; b = xb[0]
            S.dma('sp', f.t[:], mem_d[s, mt * 128:(mt + 1) * 128, :], key=f.name, writes=[f])
            S.op('act', lambda e, f=f, b=b: e.copy(b.t[:], f.t[:]), reads=[f], writes=[b])
            for c in range(8):
                S.op('pe', lambda e, c=c, b=b: e.transpose(pT[:, c * 128:(c + 1) * 128], b.t[:, c * 128:(c + 1) * 128], cb['ident'].t[:]),
                     reads=[b, cb['ident']], writes=[PB[6]])
            S.op('dve', lambda e, mt=mt: e.tensor_copy(memT[:, :, mt * 128:(mt + 1) * 128], pT.rearrange("p (c n) -> p c n", c=8)), reads=[PB[6]], writes=[QT])
        for hf in range(2):
            wk = load_w([(w_ck[:, hf * 512:(hf + 1) * 512], 0)])
            for c4 in range(4):
                mm8(PB[0].t[:, 0:256], PB[0], lambda k, wk=wk, c4=c4: wk.t[:, k, c4 * 128:(c4 + 1) * 128], lambda k: memT[:, k, :], [wk, QT])
                S.op('act', lambda e, hf=hf, c4=c4: e.copy(kcT[:, hf * 4 + c4, :], PB[0].t[:, 0:256]), reads=[PB[0]], writes=[KT])
        for hf in range(2):
            wv = load_w([(w_cv[:, hf * 512:(hf + 1) * 512], 0)])
            for mt in range(2):
                mm8(PB[1].t[:], PB[1], lambda k, mt=mt: memT[:, k, mt * 128:(mt + 1) * 128], lambda k, wv=wv: wv.t[:, k, :], [wv, QT])
                S.op('act', lambda e, hf=hf, mt=mt: e.copy(vm[:, mt, hf * 512:(hf + 1) * 512], PB[1].t[:]), reads=[PB[1]], writes=[qraw])
        for tb in range(4):
            tsl = slice(tb * 512, (tb + 1) * 512)
            for hf in range(2):
                wq = load_w([(w_cq[:, hf * 512:(hf + 1) * 512], 0)])
                for c4 in range(4):
                    mm8(PB[0].t[:], PB[0], lambda k, wq=wq, c4=c4: wq.t[:, k, c4 * 128:(c4 + 1) * 128], lambda k: xT.t[:, k, tsl], [wq, xT])
                    S.op('act', lambda e, hf=hf, c4=c4: e.copy(qcT[:, hf * 4 + c4, :], PB[0].t[:]), reads=[PB[0]], writes=[rr_a])
            for hh in range(4):
                for mt in range(2):
                    pj = PB[2 + mt]
                    for j in range(2):
                        S.op('pe', lambda e, j=j, mt=mt, hh=hh, pj=pj: e.matmul(pj.t[:], kcT[:, 2 * hh + j, mt * 128:(mt + 1) * 128], qcT[:, 2 * hh + j, :],
                                                                              start=(j == 0), stop=(j == 1)), reads=[KT, rr_a], writes=[pj])
                    S.op('act', lambda e, mt=mt, pj=pj: e.activation(PT[mt].t[:], pj.t[:], AF.Exp, scale=1.0 / 16), reads=[pj], writes=[PT[mt]])
                for mt in range(2):
                    S.op('pe', lambda e, mt=mt: e.matmul(PB[7].t[:], cb['ones'].t[:], PT[mt].t[:], start=(mt == 0), stop=(mt == 1)),
                         reads=[cb['ones'], PT[mt]], writes=[PB[7]])
                S.op('dve', lambda e: e.reciprocal(rt1.t[:], PB[7].t[:]), reads=[PB[7]], writes=[rt1])
                for j in range(2):
                    pj = PB[j]
                    for mt in range(2):
                        S.op('pe', lambda e, j=j, mt=mt, hh=hh, pj=pj: e.matmul(pj.t[:], vm[:, mt, (2 * hh + j) * 128:(2 * hh + j + 1) * 128], PT[mt].t[:],
                                                                              start=(mt == 0), stop=(mt == 1)), reads=[qraw, PT[mt]], writes=[pj])
                    S.op('dve', lambda e, j=j, hh=hh, pj=pj: e.tensor_tensor(ocT[:, 2 * hh + j, :], pj.t[:], rt1.t[:], ALU.mult), reads=[pj, rt1], writes=[onT])
            wo = [load_w([(w_co[:, hf * 512:(hf + 1) * 512], 0)]) for hf in range(2)]
            for tt in range(4):
                t = tb * 4 + tt
                f = xf[0]
                S.dma('sp', f.t[:], x1_d[s * S_LEN + t * 128:s * S_LEN + (t + 1) * 128, :], key=f.name, reads=[x1res], writes=[f])
                for hf in range(2):
                    pj = PB[4 + hf]
                    mm8(pj.t[:], pj, lambda k, tt=tt: ocT[:, k, tt * 128:(tt + 1) * 128], lambda k, hf=hf: wo[hf].t[:, k, :], [onT, wo[hf]])
                    S.op('dve', lambda e, hf=hf, pj=pj, f=f: e.scalar_tensor_tensor(lny[:, hf * 512:(hf + 1) * 512], f.t[:, hf * 512:(hf + 1) * 512], ALPHA, pj.t[:], ALU.mult, ALU.add),
                         reads=[f, pj], writes=[ang])
                layer_norm()
                S.dma('sp', x2_d[s * S_LEN + t * 128:s * S_LEN + (t + 1) * 128, :], lno, key='x2st', reads=[ang], writes=[x2res])

    def moe_stage():
        load_ln(2)
        pT = PB[6].t[:].bitcast(BF16)
        x2f = onT.t[:].rearrange("p a b -> p (a b)").bitcast(F32).rearrange("p (a b) -> p a b", a=8)
        acc = gdnT.t[:].rearrange("p a b -> p (a b)").bitcast(F32).rearrange("p (a b) -> p a b", a=8)
        xflat = xT.t[:].rearrange("p a b -> p (a b)")
        x2T = xflat[:, 0:8192].rearrange("p (a b) -> p a b", a=8)
        actT = xflat[:, 8192:16384].rearrange("p (a b) -> p a b", a=8)
        wr = S.sb("wr", [128, 8, 32], F32)
        S.dma('sp', wr.t[:], w_router.rearrange("(c p) n -> p c n", p=128), key='c_wr', writes=[wr])
        brt = S.sb("brt", [128, 32], F32)
        S.dma('sp', brt.t[:], b_router.broadcast_to([128, 32]), key='c_brt', writes=[brt])
        b1T = sinT
        b1v = sinT.t[:].bitcast(F32)[:, 0:512]
        S.dma('sp', b1v, b1T_d, key='c_b1T', writes=[b1T])
        b2a = cosT
        b2v = cosT.t[:].bitcast(F32)[0:32, :]
        S.dma('sp', b2v, b_e2, key='c_b2a', writes=[b2a])
        gates = S.sb("gates", [128, 8, 32], F32)
        gT = S.sb("gT", [32, 128], F32)
        lg = S.sb("lg", [128, 32], F32)
        ex = S.sb("ex", [128, 32], F32)
        mk = S.sb("mk", [128, 32], F32)
        m8 = S.sb("m8", [128, 8], F32)
        rsm = S.sb("rsm", [128, 4], F32)
        for blk in range(n_seq * 2):
            tok0 = blk * 1024
            for t in range(8):
                rows = slice(tok0 + t * 128, tok0 + (t + 1) * 128)
                S.dma('sp', x2f[:, t, :], x2_d[rows, :], key='x2f', reads=[x2res], writes=[onT])
                b = xb[0]
                S.op('act', lambda e, t=t, b=b: e.copy(b.t[:], x2f[:, t, :]), reads=[onT], writes=[b])
                for c in range(8):
                    S.op('pe', lambda e, c=c, b=b: e.transpose(pT[:, c * 128:(c + 1) * 128], b.t[:, c * 128:(c + 1) * 128], cb['ident'].t[:]),
                         reads=[b, cb['ident']], writes=[PB[6]])
                S.op('dve', lambda e, t=t: e.tensor_copy(x2T[:, :, t * 128:(t + 1) * 128], pT.rearrange("p (c n) -> p c n", c=8)), reads=[PB[6]], writes=[xT])
                f = xf[0]
                for g4 in range(2):
                    for c in range(4):
                        S.op('pe', lambda e, c=c, g4=g4, t=t: e.transpose(PB[7].t[:, c * 128:(c + 1) * 128], x2f[:, t, (g4 * 4 + c) * 128:(g4 * 4 + c + 1) * 128], cf['ident'].t[:]),
                             reads=[onT, cf['ident']], writes=[PB[7]])
                    S.op('act', lambda e, g4=g4, f=f: e.copy(f.t[:, g4 * 512:(g4 + 1) * 512], PB[7].t[:]), reads=[PB[7]], writes=[f])
                for k in range(8):
                    S.op('pe', lambda e, k=k, f=f: e.matmul(PB[2].t[:, 0:32], f.t[:, k * 128:(k + 1) * 128], wr.t[:, k, :], start=(k == 0), stop=(k == 7)),
                         reads=[f, wr], writes=[PB[2]])
                S.op('dve', lambda e: e.tensor_tensor(lg.t[:], PB[2].t[:, 0:32], brt.t[:], ALU.add), reads=[PB[2], brt], writes=[lg])
                S.op('dve', lambda e: e.max(m8.t[:], lg.t[:]), reads=[lg], writes=[m8])
                S.op('dve', lambda e: e.tensor_scalar(mk.t[:], lg.t[:], m8.t[:, 3:4], None, ALU.is_ge), reads=[lg, m8], writes=[mk])
                S.op('dve', lambda e: e.tensor_scalar(rsm.t[:, 0:1], m8.t[:, 0:1], -1.0, None, ALU.mult), reads=[m8], writes=[rsm])
                S.op('act', lambda e: e.activation(ex.t[:], lg.t[:], AF.Exp, bias=rsm.t[:, 0:1], scale=1.0), reads=[lg, rsm], writes=[ex])
                S.op('dve', lambda e: e.tensor_tensor(ex.t[:], ex.t[:], mk.t[:], ALU.mult), reads=[ex, mk], writes=[ex])
                S.op('dve', lambda e: e.reduce_sum(rsm.t[:, 1:2], ex.t[:], axis=AX.X), reads=[ex], writes=[rsm])
                S.op('dve', lambda e: e.reciprocal(rsm.t[:, 2:3], rsm.t[:, 1:2]), reads=[rsm], writes=[rsm])
                S.op('dve', lambda e, t=t: e.tensor_scalar(gates.t[:, t, :], ex.t[:], rsm.t[:, 2:3], None, ALU.mult), reads=[ex, rsm], writes=[gates])
                S.op('pe', lambda e, t=t: e.transpose(PB[3].t[0:32, 0:128], gates.t[:, t, :], cf['ident'].t[:]), reads=[gates, cf['ident']], writes=[PB[3]])
                S.op('act', lambda e: e.copy(gT.t[:], PB[3].t[0:32, 0:128]), reads=[PB[3]], writes=[gT])
                for hf in range(2):
                    pj = PB[4 + hf]
                    S.op('pe', lambda e, hf=hf, pj=pj: e.matmul(pj.t[:], gT.t[:], b2v[:, hf * 512:(hf + 1) * 512], start=True, stop=True), reads=[gT, b2a], writes=[pj])
                    S.op('act', lambda e, hf=hf, pj=pj, t=t: e.copy(acc[:, t, hf * 512:(hf + 1) * 512], pj.t[:]), reads=[pj], writes=[gdnT])
            for ex_i in range(32):
                for pr in range(4):
                    wb = load_w([(w_e1[ex_i][:, pr * 256:(pr + 1) * 256], 0), (w_e1[ex_i][:, 1024 + pr * 256:1024 + (pr + 1) * 256], 256)])
                    for fcl in range(2):
                        fc = pr * 2 + fcl
                        for tbk in range(2):
                            tsl = slice(tbk * 512, (tbk + 1) * 512)
                            mm8(PB[0].t[:], PB[0], lambda k, wb=wb, fcl=fcl: wb.t[:, k, fcl * 128:(fcl + 1) * 128], lambda k, tsl=tsl: x2T[:, k, tsl], [wb, xT])
                            mm8(PB[1].t[:], PB[1], lambda k, wb=wb, fcl=fcl: wb.t[:, k, 256 + fcl * 128:256 + (fcl + 1) * 128], lambda k, tsl=tsl: x2T[:, k, tsl], [wb, xT])
                            cg = ex_i * 16 + fc
                            cu = ex_i * 16 + 8 + fc
                            S.op('dve', lambda e, cg=cg: e.tensor_scalar(rt1.t[:], PB[0].t[:], b1v[:, cg:cg + 1], 7.0, ALU.add, ALU.min), reads=[PB[0], b1T], writes=[rt1])
                            S.op('dve', lambda e, cu=cu: e.tensor_scalar(rt2.t[:], PB[1].t[:], b1v[:, cu:cu + 1], 7.0, ALU.add, ALU.min), reads=[PB[1], b1T], writes=[rt2])
                            S.op('pool', lambda e: e.tensor_scalar(rt2.t[:], rt2.t[:], -7.0, 1.0, ALU.max, ALU.add), reads=[rt2], writes=[rt2])
                            S.op('act', lambda e: e.activation(dbgt.t[:], rt1.t[:], AF.Sigmoid, scale=1.702), reads=[rt1], writes=[dbgt])
                            S.op('pool', lambda e: e.tensor_tensor(rt1.t[:], rt1.t[:], dbgt.t[:], ALU.mult), reads=[rt1, dbgt], writes=[rt1])
                            S.op('pool', lambda e, fc=fc, tsl=tsl: e.tensor_tensor(actT[:, fc, tsl], rt1.t[:], rt2.t[:], ALU.mult), reads=[rt1, rt2], writes=[xT_act])
                w2 = [load_w([(w_e2[ex_i][:, hf * 512:(hf + 1) * 512], 0)]) for hf in range(2)]
                for t in range(8):
                    for hf in range(2):
                        pj = PB[4 + hf]
                        mm8(pj.t[:], pj, lambda k, t=t: actT[:, k, t * 128:(t + 1) * 128], lambda k, hf=hf: w2[hf].t[:, k, :], [xT_act, w2[hf]])
                        S.op('dve', lambda e, t=t, hf=hf, pj=pj, ex_i=ex_i: e.scalar_tensor_tensor(
                            acc[:, t, hf * 512:(hf + 1) * 512], pj.t[:], gates.t[:, t, ex_i:ex_i + 1], acc[:, t, hf * 512:(hf + 1) * 512], ALU.mult, ALU.add),
                            reads=[pj, gates, gdnT], writes=[gdnT])
            for t in range(8):
                S.op('dve', lambda e, t=t: e.scalar_tensor_tensor(lny, x2f[:, t, :], ALPHA, acc[:, t, :], ALU.mult, ALU.add), reads=[onT, gdnT], writes=[ang])
                layer_norm()
                tg = tok0 + t * 128
                S.dma('sp', out_d[tg // S_LEN, tg % S_LEN:tg % S_LEN + 128, :], lno, key='outst', reads=[ang])


    def moe_group():
        CAP = 192
        load_ln(2)
        pT = PB[6].t[:].bitcast(BF16)
        x2f = onT.t[:].rearrange("p a b -> p (a b)").bitcast(F32).rearrange("p (a b) -> p a b", a=8)
        acc = gdnT.t[:].rearrange("p a b -> p (a b)").bitcast(F32).rearrange("p (a b) -> p a b", a=8)
        accflat = gdnT.t[:].rearrange("p a b -> p (a b)").bitcast(F32)
        xflat = xT.t[:].rearrange("p a b -> p (a b)")
        x2tok = xflat[:, 0:8192].rearrange("p (a b) -> p a b", a=8)
        o0 = 8192
        xgT = xflat[:, o0:o0 + 8 * CAP].rearrange("p (a b) -> p a b", a=8); o0 += 8 * CAP
        actT = xflat[:, o0:o0 + 8 * CAP].rearrange("p (a b) -> p a b", a=8); o0 += 8 * CAP
        Pm = xflat[:, o0:o0 + 8 * CAP].rearrange("p (a b) -> p a b", a=8); o0 += 8 * CAP
        PTa = xflat[:, o0:o0 + 1024].rearrange("p (a b) -> p a b", a=8); o0 += 1024
        PTb = xflat[:, o0:o0 + 1024].rearrange("p (a b) -> p a b", a=8); o0 += 1024
        yba = xflat[:, o0:o0 + 1024]; o0 += 1024
        ybb = qraw.t[:, 0:1024]
        assert o0 <= 16384
        cview = cosT.t[:].bitcast(F32)
        iota = cview[:, 0:CAP]
        posm = cview[:, 256:512].rearrange("p (a b) -> p a b", a=8)
        S.dma('sp', iota, iota_d, key='c_iota', writes=[cosT])
        wr = S.sb("wr", [128, 8, 32], F32)
        S.dma('sp', wr.t[:], w_router.rearrange("(c p) n -> p c n", p=128), key='c_wr', writes=[wr])
        brt = S.sb("brt", [128, 32], F32)
        S.dma('sp', brt.t[:], b_router.broadcast_to([128, 32]), key='c_brt', writes=[brt])
        b1v = sinT.t[:].bitcast(F32)[:, 0:512]
        S.dma('sp', b1v, b1T_d, key='c_b1T', writes=[sinT])
        b2bc = ang.t[:, 0:1024]
        gates = S.sb("gates", [128, 8, 32], F32)
        lg = S.sb("lg", [128, 32], F32)
        ex = S.sb("ex", [128, 32], F32)
        mk = S.sb("mk", [128, 32], F32)
        offb = S.sb("offb", [128, 32], F32)
        m8 = S.sb("m8", [128, 8], F32)
        rsm = S.sb("rsm", [128, 4], F32)
        XTK = Res('x2tok'); XW = Res('moe_work'); PMR = Res('Pm'); PTR = Res('PT'); YBR = Res('yb'); XGR = Res('xgT'); ACR = Res('actT')
        for blk in range(n_seq * 2):
            tok0 = blk * 1024
            S.op('dve', lambda e: e.memset(offb.t[:], 0.0), writes=[offb])
            S.op('pool', lambda e: e.memset(accflat, 0.0), writes=[gdnT])
            for t in range(8):
                rows = slice(tok0 + t * 128, tok0 + (t + 1) * 128)
                S.dma('sp', x2f[:, t, :], x2_d[rows, :], key='x2f', reads=[x2res], writes=[onT])
                S.op('act', lambda e, t=t: e.copy(x2tok[:, t, :], x2f[:, t, :]), reads=[onT], writes=[xT if blk == 0 and t == 0 else XTK, XTK])
                f = xf[0]
                for g4 in range(2):
                    for c in range(4):
                        S.op('pe', lambda e, c=c, g4=g4, t=t: e.transpose(PB[7].t[:, c * 128:(c + 1) * 128], x2f[:, t, (g4 * 4 + c) * 128:(g4 * 4 + c + 1) * 128], cf['ident'].t[:]),
                             reads=[onT, cf['ident']], writes=[PB[7]])
                    S.op('act', lambda e, g4=g4, f=f: e.copy(f.t[:, g4 * 512:(g4 + 1) * 512], PB[7].t[:]), reads=[PB[7]], writes=[f])
                for k in range(8):
                    S.op('pe', lambda e, k=k, f=f: e.matmul(PB[2].t[:, 0:32], f.t[:, k * 128:(k + 1) * 128], wr.t[:, k, :], start=(k == 0), stop=(k == 7)),
                         reads=[f, wr], writes=[PB[2]])
                S.op('dve', lambda e: e.tensor_tensor(lg.t[:], PB[2].t[:, 0:32], brt.t[:], ALU.add), reads=[PB[2], brt], writes=[lg])
                S.op('dve', lambda e: e.max(m8.t[:], lg.t[:]), reads=[lg], writes=[m8])
                S.op('dve', lambda e: e.tensor_scalar(mk.t[:], lg.t[:], m8.t[:, 3:4], None, ALU.is_ge), reads=[lg, m8], writes=[mk])
                S.op('dve', lambda e: e.tensor_scalar(rsm.t[:, 0:1], m8.t[:, 0:1], -1.0, None, ALU.mult), reads=[m8], writes=[rsm])
                S.op('act', lambda e: e.activation(ex.t[:], lg.t[:], AF.Exp, bias=rsm.t[:, 0:1], scale=1.0), reads=[lg, rsm], writes=[ex])
                S.op('dve', lambda e: e.tensor_tensor(ex.t[:], ex.t[:], mk.t[:], ALU.mult), reads=[ex, mk], writes=[ex])
                S.op('dve', lambda e: e.reduce_sum(rsm.t[:, 1:2], ex.t[:], axis=AX.X), reads=[ex], writes=[rsm])
                S.op('dve', lambda e: e.reciprocal(rsm.t[:, 2:3], rsm.t[:, 1:2]), reads=[rsm], writes=[rsm])
                S.op('dve', lambda e, t=t: e.tensor_scalar(gates.t[:, t, :], ex.t[:], rsm.t[:, 2:3], None, ALU.mult), reads=[ex, rsm], writes=[gates])
                S.op('pe', lambda e: e.matmul(PB[3].t[:, 0:32], cf['u_strict'].t[:], mk.t[:], start=True, stop=True), reads=[cf['u_strict'], mk], writes=[PB[3]])
                S.op('pe', lambda e: e.matmul(PB[3].t[:, 32:64], cf['ones'].t[:], mk.t[:], start=True, stop=True), reads=[cf['ones'], mk], writes=[PB[3]])
                S.op('dve', lambda e, t=t: e.tensor_tensor(posm[:, t, :], PB[3].t[:, 0:32], offb.t[:], ALU.add), reads=[PB[3], offb], writes=[cosT])
                S.op('dve', lambda e: e.tensor_tensor(offb.t[:], PB[3].t[:, 32:64], offb.t[:], ALU.add), reads=[PB[3], offb], writes=[offb])
                S.op('dve', lambda e, t=t: e.scalar_tensor_tensor(posm[:, t, :], posm[:, t, :], 1.0, mk.t[:], ALU.add, ALU.mult), reads=[cosT, mk], writes=[cosT])
                S.op('dve', lambda e, t=t: e.tensor_scalar(posm[:, t, :], posm[:, t, :], -1.0, None, ALU.add), reads=[cosT], writes=[cosT])
            for ex_i in range(32):
                S.dma('sp', b2bc, b_e2[ex_i:ex_i + 1, :].broadcast_to([128, D]), key='b2bc', writes=[ang])
                for t in range(8):
                    S.op('pool' if t % 2 else 'dve', lambda e, t=t, ex_i=ex_i: e.tensor_scalar(Pm[:, t, :], iota, posm[:, t, ex_i:ex_i + 1], None, ALU.is_equal),
                         reads=[cosT], writes=[PMR])
                for t in range(8):
                    S.op('pe', lambda e, t=t: e.transpose(pT[:, t * 128:(t + 1) * 128], Pm[:, t, 0:128], cb['ident'].t[:]), reads=[PMR, cb['ident']], writes=[PB[6]])
                S.op('act', lambda e: e.copy(PTa, pT.rearrange("p (a b) -> p a b", a=8)), reads=[PB[6]], writes=[PTR])
                for t in range(8):
                    S.op('pe', lambda e, t=t: e.transpose(pT[0:64, t * 128:(t + 1) * 128], Pm[:, t, 128:192], cb['ident'].t[:]), reads=[PMR, cb['ident']], writes=[PB[6]])
                S.op('act', lambda e: e.copy(PTb[0:64], pT[0:64].rearrange("p (a b) -> p a b", a=8)), reads=[PB[6]], writes=[PTR])
                for c in range(8):
                    pj = PB[c // 2]
                    for t in range(8):
                        S.op('pe', lambda e, c=c, t=t, pj=pj: e.matmul(pj.t[:, (c % 2) * CAP:(c % 2 + 1) * CAP], x2tok[:, t, c * 128:(c + 1) * 128], Pm[:, t, :],
                                                                    start=(c % 2 == 0 and t == 0), stop=(t == 7), skip_group_check=True), reads=[XTK, PMR], writes=[pj])
                for i in range(4):
                    S.op('act' if i % 2 else 'dve', (lambda e, i=i: e.copy(xgT[:, 2 * i:2 * i + 2, :], PB[i].t[:, 0:2 * CAP].rearrange("p (a b) -> p a b", a=2))) if i % 2 else
                         (lambda e, i=i: e.tensor_copy(xgT[:, 2 * i:2 * i + 2, :], PB[i].t[:, 0:2 * CAP].rearrange("p (a b) -> p a b", a=2))), reads=[PB[i]], writes=[XGR])
                for pr in range(4):
                    wb = load_w([(w_e1[ex_i][:, pr * 256:(pr + 1) * 256], 0), (w_e1[ex_i][:, 1024 + pr * 256:1024 + (pr + 1) * 256], 256)])
                    for fcl in range(2):
                        fc = pr * 2 + fcl
                        mm8(PB[4].t[:, 0:CAP], PB[4], lambda k, wb=wb, fcl=fcl: wb.t[:, k, fcl * 128:(fcl + 1) * 128], lambda k: xgT[:, k, :], [wb, XGR])
                        mm8(PB[5].t[:, 0:CAP], PB[5], lambda k, wb=wb, fcl=fcl: wb.t[:, k, 256 + fcl * 128:256 + (fcl + 1) * 128], lambda k: xgT[:, k, :], [wb, XGR])
                        cg = ex_i * 16 + fc
                        cu = ex_i * 16 + 8 + fc
                        S.op('dve', lambda e, cg=cg: e.tensor_scalar(rt1.t[:, 0:CAP], PB[4].t[:, 0:CAP], b1v[:, cg:cg + 1], 7.0, ALU.add, ALU.min), reads=[PB[4], sinT], writes=[rt1])
                        S.op('dve', lambda e, cu=cu: e.tensor_scalar(rt2.t[:, 0:CAP], PB[5].t[:, 0:CAP], b1v[:, cu:cu + 1], 7.0, ALU.add, ALU.min), reads=[PB[5], sinT], writes=[rt2])
                        S.op('pool', lambda e: e.tensor_scalar(rt2.t[:, 0:CAP], rt2.t[:, 0:CAP], -7.0, 1.0, ALU.max, ALU.add), reads=[rt2], writes=[rt2])
                        S.op('act', lambda e: e.activation(dbgt.t[:, 0:CAP], rt1.t[:, 0:CAP], AF.Sigmoid, scale=1.702), reads=[rt1], writes=[dbgt])
                        S.op('pool', lambda e: e.tensor_tensor(rt1.t[:, 0:CAP], rt1.t[:, 0:CAP], dbgt.t[:, 0:CAP], ALU.mult), reads=[rt1, dbgt], writes=[rt1])
                        S.op('pool', lambda e, fc=fc: e.tensor_tensor(actT[:, fc, :], rt1.t[:, 0:CAP], rt2.t[:, 0:CAP], ALU.mult), reads=[rt1, rt2], writes=[ACR])
                w2 = [load_w([(w_e2[ex_i][:, hf * 512:(hf + 1) * 512], 0)]) for hf in range(2)]
                for (s0, sn, ydst) in ((0, 128, yba), (128, 64, ybb)):
                    for hf in range(2):
                        pj = PB[hf]
                        mm8(pj.t[0:sn, :], pj, lambda k, s0=s0, sn=sn: actT[:, k, s0:s0 + sn], lambda k, hf=hf: w2[hf].t[:, k, :], [ACR, w2[hf]])
                        S.op('dve', lambda e, hf=hf, pj=pj, sn=sn, ydst=ydst: e.tensor_tensor(ydst[0:sn, hf * 512:(hf + 1) * 512], pj.t[0:sn, :], b2bc[0:sn, hf * 512:(hf + 1) * 512], ALU.add),
                             reads=[pj, ang], writes=[YBR])
                for t in range(8):
                    for hf in range(2):
                        pj = PB[2 + hf]
                        S.op('pe', lambda e, t=t, hf=hf, pj=pj: e.matmul(pj.t[:], PTa[:, t, :], yba[:, hf * 512:(hf + 1) * 512], start=True, stop=False), reads=[PTR, YBR], writes=[pj])
                        S.op('pe', lambda e, t=t, hf=hf, pj=pj: e.matmul(pj.t[:], PTb[0:64, t, :], ybb[0:64, hf * 512:(hf + 1) * 512], start=False, stop=True), reads=[PTR, YBR], writes=[pj])
                        S.op('dve', lambda e, t=t, hf=hf, pj=pj, ex_i=ex_i: e.scalar_tensor_tensor(
                            acc[:, t, hf * 512:(hf + 1) * 512], pj.t[:], gates.t[:, t, ex_i:ex_i + 1], acc[:, t, hf * 512:(hf + 1) * 512], ALU.mult, ALU.add),
                            reads=[pj, gates, gdnT], writes=[gdnT])
            for t in range(8):
                S.op('dve', lambda e, t=t: e.scalar_tensor_tensor(lny, x2f[:, t, :], ALPHA, acc[:, t, :], ALU.mult, ALU.add), reads=[onT, gdnT], writes=[ang])
                layer_norm()
                tg = tok0 + t * 128
                S.dma('sp', out_d[tg // S_LEN, tg % S_LEN:tg % S_LEN + 128, :], lno, key='outst', reads=[ang])


    def dbg_x2(t):
        return gdnT.t[:].rearrange("p a b -> p (a b)").bitcast(F32)[:, (t % 8) * 1024:(t % 8 + 1) * 1024]

    def moe_sparse():
        NTT = n_seq * NT
        CT = -(-5 * n_seq // 2)
        C = CT * 128
        NR = 32 * C
        pT = PB[6].t[:].bitcast(BF16)
        Xg = nc.dram_tensor("xg_scr", [NR, D], BF16, kind="Internal").ap()
        Yg = nc.dram_tensor("yg_scr", [NR, D], F32, kind="Internal").ap()
        XgR = Res('XgR'); YgR = Res('YgR')
        eC_d = dram_in("eC", [128, 32])
        xflat = xT.t[:].rearrange("p a b -> p (a b)")
        oflat = onT.t[:].rearrange("p a b -> p (a b)")
        gflat32 = gdnT.t[:].rearrange("p a b -> p (a b)").bitcast(F32)
        oflat32 = oflat.bitcast(F32)
        xgT = xflat[:, 0:8 * C].rearrange("p (a b) -> p a b", a=8)
        actT = oflat[:, 0:8 * C].rearrange("p (a b) -> p a b", a=8)
        wr = S.sb("wr", [128, 8, 32], F32)
        S.dma('sp', wr.t[:], w_router.rearrange("(c p) n -> p c n", p=128), key='c_wr', writes=[wr])
        brt = S.sb("brt", [128, 32], F32)
        S.dma('sp', brt.t[:], b_router.broadcast_to([128, 32]), key='c_brt', writes=[brt])
        eC = S.sb("eCt", [128, 32], F32)
        S.dma('sp', eC.t[:], eC_d, key='c_eC', writes=[eC])
        b1v = sinT.t[:].bitcast(F32)[:, 0:512]
        S.dma('sp', b1v, b1T_d, key='c_b1T', writes=[sinT])
        slots = S.sb("slots", [128, NTT, 4], I32)
        gate4 = S.sb("gate4", [128, NTT, 4], F32)
        off = S.sb("off", [128, 32], F32)
        S.op('dve', lambda e: e.memset(off.t[:], 0.0), writes=[off])
        lg = S.sb("lg", [128, 32], F32)
        mk = S.sb("mk", [128, 32], F32)
        posf = S.sb("posf", [128, 32], F32)
        tmp32 = S.sb("tmp32", [128, 32], F32)
        m8 = S.sb("m8", [128, 8], F32)
        rsm = S.sb("rsm", [128, 16], F32)
        ZW = 8 * C if 8 * C <= 16384 else 16384
        S.op('dve', lambda e: e.memset(xflat[:, 0:ZW], 0.0), reads=[], writes=[xT])
        rows_per_dma = 128 * (ZW // 1024)
        r0 = 0
        while r0 < NR:
            nrow = min(rows_per_dma, NR - r0)
            jj = nrow // 128
            S.dma('sp', Xg[r0:r0 + nrow, :].rearrange("(p j) d -> p (j d)", p=128), xflat[:, 0:jj * 1024], key='xgz', reads=[xT], writes=[XgR])
            r0 += nrow
        xbm = [(kT, kT.t[:, 0:1024]), (qT, qT.t[:, 0:1024])]
        for t in range(NTT):
            rows = slice(t * 128, (t + 1) * 128)
            f = xf[0]
            bres, bap = xbm[t % 2]
            S.dma('sp', dbg_x2(t), x2_d[rows, :], key='x2f', reads=[x2res], writes=[gdnT])
            x2t = dbg_x2(t)
            S.op('act', lambda e, bap=bap, x2t=x2t: e.copy(bap, x2t), reads=[gdnT], writes=[bres])
            for g4 in range(2):
                for c in range(4):
                    S.op('pe', lambda e, c=c, g4=g4, x2t=x2t: e.transpose(PB[7].t[:, c * 128:(c + 1) * 128], x2t[:, (g4 * 4 + c) * 128:(g4 * 4 + c + 1) * 128], cf['ident'].t[:]),
                         reads=[gdnT, cf['ident']], writes=[PB[7]])
                S.op('act', lambda e, g4=g4, f=f: e.copy(f.t[:, g4 * 512:(g4 + 1) * 512], PB[7].t[:]), reads=[PB[7]], writes=[f])
            for k in range(8):
                S.op('pe', lambda e, k=k, f=f: e.matmul(PB[2].t[:, 0:32], f.t[:, k * 128:(k + 1) * 128], wr.t[:, k, :], start=(k == 0), stop=(k == 7)),
                     reads=[f, wr], writes=[PB[2]])
            S.op('dve', lambda e: e.tensor_tensor(lg.t[:], PB[2].t[:, 0:32], brt.t[:], ALU.add), reads=[PB[2], brt], writes=[lg])
            S.op('dve', lambda e: e.max(m8.t[:], lg.t[:]), reads=[lg], writes=[m8])
            S.op('dve', lambda e: e.tensor_scalar(mk.t[:], lg.t[:], m8.t[:, 3:4], None, ALU.is_ge), reads=[lg, m8], writes=[mk])
            S.op('pe', lambda e: e.matmul(PB[3].t[:, 0:32], cf['u_strict'].t[:], mk.t[:], start=True, stop=True), reads=[cf['u_strict'], mk], writes=[PB[3]])
            S.op('pe', lambda e: e.matmul(PB[3].t[:, 32:64], cf['ones'].t[:], mk.t[:], start=True, stop=True), reads=[cf['ones'], mk], writes=[PB[3]])
            S.op('dve', lambda e: e.tensor_tensor(posf.t[:], PB[3].t[:, 0:32], off.t[:], ALU.add), reads=[PB[3], off], writes=[posf])
            S.op('dve', lambda e: e.tensor_tensor(off.t[:], PB[3].t[:, 32:64], off.t[:], ALU.add), reads=[PB[3], off], writes=[off])
            S.op('dve', lambda e: e.tensor_scalar(posf.t[:], posf.t[:], float(C - 1), None, ALU.min), reads=[posf], writes=[posf])
            S.op('dve', lambda e: e.tensor_tensor(posf.t[:], posf.t[:], eC.t[:], ALU.add), reads=[posf, eC], writes=[posf])
            for k in range(4):
                S.op('dve', lambda e, k=k: e.scalar_tensor_tensor(tmp32.t[:], lg.t[:], m8.t[:, k:k + 1], posf.t[:], ALU.is_equal, ALU.mult), reads=[lg, m8, posf], writes=[tmp32])
                S.op('dve', lambda e, k=k: e.reduce_sum(rsm.t[:, 8 + k:9 + k], tmp32.t[:], axis=AX.X), reads=[tmp32], writes=[rsm])
            S.op('dve', lambda e, t=t: e.tensor_copy(slots.t[:, t, :], rsm.t[:, 8:12]), reads=[rsm], writes=[slots])
            S.op('dve', lambda e: e.tensor_scalar(rsm.t[:, 0:1], m8.t[:, 0:1], -1.0, None, ALU.mult), reads=[m8], writes=[rsm])
            S.op('act', lambda e: e.activation(rsm.t[:, 4:8], m8.t[:, 0:4], AF.Exp, bias=rsm.t[:, 0:1], scale=1.0), reads=[m8, rsm], writes=[rsm])
            S.op('dve', lambda e: e.reduce_sum(rsm.t[:, 1:2], rsm.t[:, 4:8], axis=AX.X), reads=[rsm], writes=[rsm])
            S.op('dve', lambda e: e.reciprocal(rsm.t[:, 2:3], rsm.t[:, 1:2]), reads=[rsm], writes=[rsm])
            S.op('dve', lambda e, t=t: e.tensor_scalar(gate4.t[:, t, :], rsm.t[:, 4:8], rsm.t[:, 2:3], None, ALU.mult), reads=[rsm], writes=[gate4])
            for k in range(4):
                S.dma_fn('pool', lambda e, t=t, k=k, bap=bap: e.indirect_dma_start(
                    out=Xg[:, :], out_offset=bass.IndirectOffsetOnAxis(ap=slots.t[:, t, k:k + 1], axis=0), in_=bap, in_offset=None,
                    bounds_check=NR - 1, oob_is_err=False), key='scat' + bres.name, reads=[bres, slots, XgR], writes=[XgR])
        blks = []
        c0 = 0
        while c0 < C:
            blks.append((c0, min(512, C - c0)))
            c0 += 512
        xgb = [(qraw, qraw.t[:, 0:1024]), (kTm[0], kTm[0].t[:, 0:1024])]
        ygb = [(ang, ang.t[:, 0:1024]), (rr_a, rr_a.t[:, 0:1024])]
        b2bc = posi.t[:].bitcast(F32)[:, 0:1024]
        for ex_i in range(32):
            S.dma('sp', b2bc, b_e2[ex_i:ex_i + 1, :].broadcast_to([128, D]), key='b2bc', writes=[posi])
            for r in range(CT):
                gres, gap = xgb[r % 2]
                S.dma('sp', gap, Xg[ex_i * C + r * 128:ex_i * C + (r + 1) * 128, :], key='xgl' + gres.name, reads=[XgR], writes=[gres])
                for c in range(8):
                    S.op('pe', lambda e, c=c, gap=gap: e.transpose(pT[:, c * 128:(c + 1) * 128], gap[:, c * 128:(c + 1) * 128], cb['ident'].t[:]),
                         reads=[gres, cb['ident']], writes=[PB[6]])
                S.op('dve', lambda e, r=r: e.tensor_copy(xgT[:, :, r * 128:(r + 1) * 128], pT.rearrange("p (c n) -> p c n", c=8)), reads=[PB[6]], writes=[xT])
            for pr in range(4):
                wb = load_w([(w_e1[ex_i][:, pr * 256:(pr + 1) * 256], 0), (w_e1[ex_i][:, 1024 + pr * 256:1024 + (pr + 1) * 256], 256)])
                for fcl in range(2):
                    fc = pr * 2 + fcl
                    for (b0, bn) in blks:
                        tsl = slice(b0, b0 + bn)
                        mm8(PB[0].t[:, 0:bn], PB[0], lambda k, wb=wb, fcl=fcl: wb.t[:, k, fcl * 128:(fcl + 1) * 128], lambda k, tsl=tsl: xgT[:, k, tsl], [wb, xT])
                        mm8(PB[1].t[:, 0:bn], PB[1], lambda k, wb=wb, fcl=fcl: wb.t[:, k, 256 + fcl * 128:256 + (fcl + 1) * 128], lambda k, tsl=tsl: xgT[:, k, tsl], [wb, xT])
                        cg = ex_i * 16 + fc
                        cu = ex_i * 16 + 8 + fc
                        S.op('dve', lambda e, cg=cg, bn=bn: e.tensor_scalar(rt1.t[:, 0:bn], PB[0].t[:, 0:bn], b1v[:, cg:cg + 1], 7.0, ALU.add, ALU.min), reads=[PB[0], sinT], writes=[rt1])
                        S.op('dve', lambda e, cu=cu, bn=bn: e.tensor_scalar(rt2.t[:, 0:bn], PB[1].t[:, 0:bn], b1v[:, cu:cu + 1], 7.0, ALU.add, ALU.min), reads=[PB[1], sinT], writes=[rt2])
                        S.op('pool', lambda e, bn=bn: e.tensor_scalar(rt2.t[:, 0:bn], rt2.t[:, 0:bn], -7.0, 1.0, ALU.max, ALU.add), reads=[rt2], writes=[rt2])
                        S.op('act', lambda e, bn=bn: e.activation(dbgt.t[:, 0:bn], rt1.t[:, 0:bn], AF.Sigmoid, scale=1.702), reads=[rt1], writes=[dbgt])
                        S.op('pool', lambda e, bn=bn: e.tensor_tensor(rt1.t[:, 0:bn], rt1.t[:, 0:bn], dbgt.t[:, 0:bn], ALU.mult), reads=[rt1, dbgt], writes=[rt1])
                        S.op('pool', lambda e, fc=fc, tsl=tsl, bn=bn: e.tensor_tensor(actT[:, fc, tsl], rt1.t[:, 0:bn], rt2.t[:, 0:bn], ALU.mult), reads=[rt1, rt2], writes=[onT])
            w2 = [load_w([(w_e2[ex_i][:, hf * 512:(hf + 1) * 512], 0)]) for hf in range(2)]
            for r in range(CT):
                yres, yap = ygb[r % 2]
                for hf in range(2):
                    pj = PB[4 + hf]
                    mm8(pj.t[:], pj, lambda k, r=r: actT[:, k, r * 128:(r + 1) * 128], lambda k, hf=hf: w2[hf].t[:, k, :], [onT, w2[hf]])
                    S.op('dve', lambda e, hf=hf, pj=pj, yap=yap: e.tensor_tensor(yap[:, hf * 512:(hf + 1) * 512], pj.t[:], b2bc[:, hf * 512:(hf + 1) * 512], ALU.add),
                         reads=[pj, posi], writes=[yres])
                S.dma('sp', Yg[ex_i * C + r * 128:ex_i * C + (r + 1) * 128, :], yap, key='ygst' + yres.name, reads=[yres], writes=[YgR])
        load_ln(2)
        ysets = [(gdnT, gflat32), (onT, oflat32)]
        for t in range(NTT):
            yres, yflat = ysets[t % 2]
            for k in range(4):
                S.dma_fn('pool', lambda e, t=t, k=k, yflat=yflat: e.indirect_dma_start(
                    out=yflat[:, k * 1024:(k + 1) * 1024], out_offset=None, in_=Yg[:, :],
                    in_offset=bass.IndirectOffsetOnAxis(ap=slots.t[:, t, k:k + 1], axis=0), bounds_check=NR - 1, oob_is_err=False),
                    key='gath' + yres.name, reads=[YgR, slots], writes=[yres])
            f = xf[0]
            S.dma('sp', f.t[:], x2_d[t * 128:(t + 1) * 128, :], key=f.name, reads=[x2res], writes=[f])
            S.op('dve', lambda e, t=t, yflat=yflat: e.tensor_scalar(lny, yflat[:, 0:1024], gate4.t[:, t, 0:1], None, ALU.mult), reads=[yres, gate4], writes=[ang])
            for k in range(1, 4):
                S.op('dve', lambda e, t=t, k=k, yflat=yflat: e.scalar_tensor_tensor(lny, yflat[:, k * 1024:(k + 1) * 1024], gate4.t[:, t, k:k + 1], lny, ALU.mult, ALU.add),
                     reads=[yres, gate4, ang], writes=[ang])
            S.op('dve', lambda e, f=f: e.scalar_tensor_tensor(lny, f.t[:], ALPHA, lny, ALU.mult, ALU.add), reads=[f, ang], writes=[ang])
            layer_norm()
            tg = t * 128
            S.dma('sp', out_d[tg // S_LEN, tg % S_LEN:tg % S_LEN + 128, :], lno, key='outst', reads=[ang])


    try:
      for s in range(n_seq):
          for t in range(NT):
              f = xf[0]
              b = xb[0]
              S.dma('sp', f.t[:], x_d[s, t * 128:(t + 1) * 128, :], key=f.name, writes=[f])
              S.op('act', lambda e, f=f, b=b: e.copy(b.t[:], f.t[:]), reads=[f], writes=[b])
              pT = PB[6].t[:].bitcast(BF16)
              for c in range(8):
                  S.op('pe', lambda e, c=c, b=b, pT=pT: e.transpose(pT[:, c * 128:(c + 1) * 128], b.t[:, c * 128:(c + 1) * 128], cb['ident'].t[:]),
                       reads=[b, cb['ident']], writes=[PB[6]])
              S.op('dve', lambda e, t=t, pT=pT: e.tensor_copy(xT.t[:, :, t * 128:(t + 1) * 128], pT.rearrange("p (c n) -> p c n", c=8)),
                   reads=[PB[6]], writes=[xT])

          stop('xT', lambda: xT.t[:, 0, 0:512], [xT])
          S.dma('sp', posi.t[:, 0:S_LEN], pos_d[s:s + 1, :].broadcast_to([128, S_LEN]), key='posi', writes=[posi])
          S.op('dve', lambda e: e.tensor_copy(ang.t[:, 0:S_LEN], posi.t[:, 0:S_LEN]), reads=[posi], writes=[ang])
          S.op('dve', lambda e: e.tensor_scalar(ang.t[:, 0:S_LEN], ang.t[:, 0:S_LEN], rcols.t[:, 0:1], None, ALU.mult), reads=[ang, rcols], writes=[ang])
          pf = posi.t[:, 0:S_LEN].bitcast(F32)
          for tab, sh, scol in ((sinT, 0.0, rcols.t[:, 1:2]), (cosT, 0.5 * PI, 1.0)):
              S.op('dve', lambda e, sh=sh: e.tensor_scalar(rr_a.t[:, 0:S_LEN], ang.t[:, 0:S_LEN], sh, None, ALU.add), reads=[ang], writes=[rr_a])
              S.op('dve', lambda e: e.tensor_scalar(posi.t[:, 0:S_LEN], rr_a.t[:, 0:S_LEN], 1.0 / (2 * PI), None, ALU.mult), reads=[rr_a], writes=[posi])
              S.op('dve', lambda e: e.tensor_copy(pf, posi.t[:, 0:S_LEN]), reads=[posi], writes=[posi])
              S.op('dve', lambda e: e.scalar_tensor_tensor(rr_a.t[:, 0:S_LEN], pf, -2 * PI, rr_a.t[:, 0:S_LEN], ALU.mult, ALU.add), reads=[posi, rr_a], writes=[rr_a])
              S.op('dve', lambda e: e.tensor_scalar(pf, rr_a.t[:, 0:S_LEN], PI, None, ALU.is_gt), reads=[rr_a], writes=[posi])
              S.op('dve', lambda e: e.scalar_tensor_tensor(rr_a.t[:, 0:S_LEN], pf, -2 * PI, rr_a.t[:, 0:S_LEN], ALU.mult, ALU.add), reads=[posi, rr_a], writes=[rr_a])
              S.op('dve', lambda e: e.tensor_scalar(rr_a.t[:, 0:S_LEN], rr_a.t[:, 0:S_LEN], -PI, PI, ALU.max, ALU.min), reads=[rr_a], writes=[rr_a])
              S.op('act', lambda e, tab=tab, scol=scol: e.activation(tab.t[:], rr_a.t[:, 0:S_LEN], AF.Sin, scale=scol), reads=[rr_a, rcols], writes=[tab])

          stop('sin', lambda: sinT.t[:, 0:512], [sinT])
          stop('cos', lambda: cosT.t[:, 1536:2048], [cosT])
          for h in range(8):
              wb = load_w([(w_in[:, O_DQ + h * 128:O_DQ + (h + 1) * 128], 0),
                           (w_in[:, O_DK + h * 128:O_DK + (h + 1) * 128], 128),
                           (w_in[:, O_DV + h * 128:O_DV + (h + 1) * 128], 256)])
              for which, dst in ((0, qT), (1, kT)):
                  for tb in range(4):
                      pj = PB[tb % 2]
                      tsl = slice(tb * 512, (tb + 1) * 512)
                      for c in range(8):
                          S.op('pe', lambda e, c=c, pj=pj, tsl=tsl, which=which, wb=wb: e.matmul(
                              pj.t[:], wb.t[:, c, which * 128:(which + 1) * 128], xT.t[:, c, tsl], start=(c == 0), stop=(c == 7)),
                              reads=[wb, xT], writes=[pj])
                      qr = qraw.sub(tb)
                      S.op('act', lambda e, pj=pj, tsl=tsl: e.copy(qraw.t[:, tsl], pj.t[:]), reads=[pj], writes=[qr])
                      S.op('pe', lambda e, tsl=tsl: e.matmul(PB[7].t[:], cb['rope_pm'].t[:], qraw.t[:, tsl], start=True, stop=True),
                           reads=[qr, cb['rope_pm']], writes=[PB[7]])
                      S.op('pool', lambda e, tsl=tsl: e.tensor_tensor(rt1.t[:], qraw.t[:, tsl], cosT.t[:, tsl], ALU.mult),
                           reads=[qr, cosT], writes=[rt1])
                      S.op('dve', lambda e, tsl=tsl: e.tensor_tensor(rt2.t[:], PB[7].t[:], sinT.t[:, tsl], ALU.mult),
                           reads=[PB[7], sinT], writes=[rt2])
                      S.op('dve', lambda e, tsl=tsl, dst=dst: e.tensor_tensor(dst.t[:, tsl], rt1.t[:], rt2.t[:], ALU.add),
                           reads=[rt1, rt2], writes=[dst.sub(tb)])
              stop('qT', lambda: qT.t[:, 0:512], [qT.sub(0)])
              stop('kT', lambda: kT.t[:, 1536:2048], [kT.sub(3)])
              for g in range(4):
                  pj = PB[g % 2]
                  for tt in range(4):
                      t = g * 4 + tt
                      for c in range(8):
                          S.op('pe', lambda e, c=c, pj=pj, t=t, tt=tt, wb=wb: e.matmul(
                              pj.t[:, tt * 128:(tt + 1) * 128], xT.t[:, c, t * 128:(t + 1) * 128], wb.t[:, c, 256:384],
                              start=(c == 0), stop=(c == 7)), reads=[wb, xT], writes=[pj])
                  S.op('act', lambda e, pj=pj, g=g: e.copy(vaug.t[:, g * 4:(g + 1) * 4, 0:128], pj.t[:].rearrange("p (a b) -> p a b", a=4)),
                       reads=[pj], writes=[vaug])
              stop('v', lambda: vaug.t[:, 0:3, :].rearrange('p a b -> p (a b)')[:, 0:396], [vaug])
              for comp in range(2):
                  S.op('pool', lambda e, comp=comp: e.tensor_scalar(kTm[comp].t[:], kT.t[:], cmask.t[:, comp:comp + 1], None, ALU.mult),
                       reads=[kT.sub(0), kT.sub(1), kT.sub(2), kT.sub(3), cmask], writes=[kTm[comp]])
              pctr = 0
              for qb in range(8):
                  qsl = slice(qb * 256, (qb + 1) * 256)
                  nk = 2 * qb + 2
                  for kt in range(nk):
                      sc = PB[2 + (pctr % 2)]
                      pt = PT[pctr % 2]
                      pctr += 1
                      ksl = slice(kt * 128, (kt + 1) * 128)
                      for comp in range(2):
                          S.op('pe', lambda e, comp=comp, sc=sc, ksl=ksl, qsl=qsl: e.matmul(
                              sc.t[:, comp * 256:(comp + 1) * 256], kTm[comp].t[:, ksl], qT.t[:, qsl],
                              start=True, stop=True), reads=[kTm[comp], qT.sub(qb // 2)], writes=[sc])
                      S.op('act', lambda e, sc=sc, pt=pt: e.activation(pt.t[:], sc.t[:], AF.Exp, scale=0.125), reads=[sc], writes=[pt])
                      subs = [0, 1]
                      if kt == 2 * qb:
                          for comp in range(2):
                              S.op('pool', lambda e, pt=pt, comp=comp: e.tensor_tensor(
                                  pt.t[:, comp * 256:comp * 256 + 128], pt.t[:, comp * 256:comp * 256 + 128], cb['u_incl'].t[:], ALU.mult),
                                  reads=[pt, cb['u_incl']], writes=[pt])
                      if kt == 2 * qb + 1:
                          subs = [1]
                          for comp in range(2):
                              S.op('pool', lambda e, pt=pt, comp=comp: e.tensor_tensor(
                                  pt.t[:, comp * 256 + 128:comp * 256 + 256], pt.t[:, comp * 256 + 128:comp * 256 + 256], cb['u_incl'].t[:], ALU.mult),
                                  reads=[pt, cb['u_incl']], writes=[pt])
                      stop('pt0', lambda pt=pt: pt.t[:], [pt])
                      for sub in subs:
                          last = 2 * qb + sub
                          for comp in range(2):
                              oacc = PB[4 + comp]
                              S.op('pe', lambda e, comp=comp, sub=sub, pt=pt, kt=kt, oacc=oacc, last=last: e.matmul(
                                  oacc.t[:, sub * 132:sub * 132 + 129], pt.t[:, comp * 256 + sub * 128:comp * 256 + (sub + 1) * 128],
                                  vaug.t[:, kt, 0:129], start=(kt == 0 and sub == 0), stop=(kt == last), skip_group_check=True),
                                  reads=[pt, vaug], writes=[oacc])
                  stop('o0', lambda: PB[4].t[:, 0:264], [PB[4]])
                  for sub in range(2):
                      t = 2 * qb + sub
                      o1 = PB[4].t[:, sub * 132:sub * 132 + 129]
                      o2 = PB[5].t[:, sub * 132:sub * 132 + 129]
                      S.op('dve', lambda e, o1=o1: e.reciprocal(osm.t[:, 0:1], o1[:, 128:129]), reads=[PB[4]], writes=[osm])
                      S.op('dve', lambda e, o2=o2: e.reciprocal(osm.t[:, 1:2], o2[:, 128:129]), reads=[PB[5]], writes=[osm])
                      S.op('dve', lambda e: e.tensor_tensor(osm.t[:, 2:3], osm.t[:, 1:2], NEG_LAM, ALU.mult), reads=[osm, lam_s], writes=[osm])
                      S.op('act', lambda e, o1=o1: e.activation(otmp.t[:], o1[:, 0:128], AF.Copy, scale=osm.t[:, 0:1]), reads=[PB[4], osm], writes=[otmp])
                      S.op('dve', lambda e, o2=o2: e.scalar_tensor_tensor(ocmb.t[:], o2[:, 0:128], osm.t[:, 2:3], otmp.t[:], ALU.mult, ALU.add),
                           reads=[PB[5], osm, otmp], writes=[ocmb])
                      S.op('act', lambda e: e.activation(ojunk.t[:], ocmb.t[:], AF.Square, accum_out=osm.t[:, 3:4]), reads=[ocmb], writes=[ojunk, osm])
                      S.op('act', lambda e: e.activation(osm.t[:, 4:5], osm.t[:, 3:4], AF.Ln, bias=eps6.t[:, 0:1], scale=1.0 / 128), reads=[osm, eps6], writes=[osm])
                      S.op('act', lambda e: e.activation(osm.t[:, 5:6], osm.t[:, 4:5], AF.Exp, scale=-0.5), reads=[osm], writes=[osm])
                      S.op('dve', lambda e: e.scalar_tensor_tensor(ontok.t[:], ocmb.t[:], osm.t[:, 5:6], gsub.t[:], ALU.mult, ALU.mult),
                           reads=[ocmb, osm, gsub], writes=[ontok])
                      stop('on0', lambda: ontok.t[:], [ontok])
                      pT = PB[6].t[:].bitcast(BF16)
                      S.op('pe', lambda e, pT=pT: e.transpose(pT[:, 0:128], ontok.t[:], cb['ident'].t[:]), reads=[ontok, cb['ident']], writes=[PB[6]])
                      S.op('act', lambda e, pT=pT, h=h, t=t: e.copy(onT.t[:, h, t * 128:(t + 1) * 128], pT[:, 0:128]), reads=[PB[6]], writes=[onT])

          if 'gdn' in stages:
              gdn_stage(s)
          if 'mix' in stages:
              mix_stage(s)
              cross_stage(s)
          if 'ogdn' in dbg and s == 0:
              for h in range(8):
                  for tb in range(4):
                      S.op('dve', lambda e, h=h, tb=tb: e.tensor_copy(dbgt.t[:], gdnT.t[:, h, tb * 512:(tb + 1) * 512]), reads=[gdnT], writes=[dbgt])
                      S.dma('sp', dbg_d['ogdn'][h * 128:(h + 1) * 128, tb * 512:(tb + 1) * 512], dbgt.t[:], key='dbgt', reads=[dbgt])
          if 'ondiff' in dbg and s == 0:
              for h in range(8):
                  for tb in range(4):
                      S.op('dve', lambda e, h=h, tb=tb: e.tensor_copy(dbgt.t[:], onT.t[:, h, tb * 512:(tb + 1) * 512]), reads=[onT], writes=[dbgt])
                      S.dma('sp', dbg_d['ondiff'][h * 128:(h + 1) * 128, tb * 512:(tb + 1) * 512], dbgt.t[:], key='dbgt', reads=[dbgt])

    except StopBuild:
        pass
    if 'mix' in stages:
        if 'moe_dense' in stages:
            moe_stage()
        elif 'moe_group' in stages:
            moe_group()
        else:
            moe_sparse()
    S.finish()
    return nc


ALL_STAGES = ('diff', 'gdn', 'mix', 'moe_group')


def make_maps(inp, n_seq, n_cores):
    consts = make_consts()
    g = lambda k: np.ascontiguousarray(np.asarray(inp[k]))
    shared = {
        "w_in": g('w_in')[0],
        "lamv": np.stack([g('diff_lambda_q1')[0], g('diff_lambda_k1')[0], g('diff_lambda_q2')[0], g('diff_lambda_k2')[0]]),
        "subln_g": g('diff_subln_g'), "w_diff_o": g('w_diff_o')[0],
        "conv_wT": np.ascontiguousarray(g('gdn_conv_w')[0].reshape(4, 24, 128).transpose(2, 1, 0).reshape(128, 96)),
        "gdn_sc": np.stack([g('gdn_A_log')[0], g('gdn_dt_bias')[0]]), "gdn_norm_g": g('gdn_norm_g'),
        "w_gdn_o": g('w_gdn_o')[0], "w_mix_o": g('w_mix_o')[0],
        "lnp": np.stack([g('ln1_g')[0], g('ln1_b')[0], g('ln2_g')[0], g('ln2_b')[0], g('ln3_g')[0], g('ln3_b')[0]]),
        "w_cq": g('w_cq')[0], "w_ck": g('w_ck')[0], "w_cv": g('w_cv')[0], "w_co": g('w_co')[0],
        "w_router": g('w_router')[0], "b_router": g('b_router'),
        "w_e1": g('w_exp_in')[0],
        "b1T": np.ascontiguousarray(g('b_exp_in')[0].reshape(32, 16, 128).transpose(2, 0, 1).reshape(128, 512)),
        "w_e2": g('w_exp_out')[0], "b_e2": g('b_exp_out')[0],
    }
    CT = -(-5 * n_seq // 2)
    shared["iota192"] = np.tile(np.arange(192, dtype=np.float32)[None, :], (128, 1))
    shared["eC"] = np.tile((np.arange(32, dtype=np.float32) * (CT * 128))[None, :], (128, 1)).astype(np.float32)
    for n in CONST_NAMES:
        shared["c_" + n] = consts[n]
    shared["c_rope_cols"] = consts['rope_cols']
    x, mem, pos = g('x'), g('mem'), g('positions').astype(np.int32)
    maps = []
    for c in range(n_cores):
        m = dict(shared)
        m["x"] = x[c * n_seq:(c + 1) * n_seq]
        m["mem"] = mem[c * n_seq:(c + 1) * n_seq]
        m["pos"] = pos[c * n_seq:(c + 1) * n_seq]
        maps.append(m)
    return maps


def kernel(**inputs):
    nc = build(n_seq=SEQ_PER_CORE, stages=ALL_STAGES)
    maps = make_maps(inputs, SEQ_PER_CORE, NCORES)
    res = run_bass_kernel_spmd(nc, maps, core_ids=list(range(NCORES)))
    out = np.concatenate([np.asarray(r["out"]) for r in res.results], axis=0)
    return out.astype(np.float32)
```
